# Optimizing a Trainium2 kernel written in Bass

```python
import math
import jax, jax.numpy as jnp
from jax import lax
import numpy as np

D_MODEL = 2048
BATCH = 8
SEQ = 2048
DEPTH = 1

EPS = 1e-6
D_ATTN = D_MODEL // 2
ATTN_HEAD_DIM = 64
N_ATTN_HEADS = D_ATTN // ATTN_HEAD_DIM
DILATED_CONFIGS = ((128, 1), (512, 4), (2048, 16))
N_BUCKETS = 32
MAX_EXACT = N_BUCKETS // 2
MAX_DISTANCE = 2048
N_GLA_HEADS = 4
D_GLA_V = D_MODEL // 2
D_GLA_K = D_GLA_V // 2
GLA_DK = D_GLA_K // N_GLA_HEADS
GLA_DV = D_GLA_V // N_GLA_HEADS
GLA_GATE_RANK = 16
GLA_GATE_TEMP = 16.0
GLA_CHUNK = 64
D_MIX = D_ATTN + D_GLA_V
IN_SPLITS = (D_ATTN, D_ATTN, D_ATTN, D_GLA_K, D_GLA_K, D_GLA_V, GLA_GATE_RANK, D_GLA_V)
D_IN = D_ATTN * 3 + D_GLA_K * 2 + D_GLA_V * 2 + GLA_GATE_RANK
PEER_HEADS = 8
PEER_N_KEYS = 128
PEER_N_EXPERTS = PEER_N_KEYS * PEER_N_KEYS
PEER_TOPK = 16
PEER_QUERY_DIM = 256
PEER_TOKEN_BLOCK = 128

kernel_name = "hybrid_dilated_gla_peer_block"


def rms_norm(x, g):
    xf = x.astype(jnp.float32)
    y = xf * lax.rsqrt(jnp.mean(xf * xf, axis=-1, keepdims=True) + EPS)
    return (y * g.astype(jnp.float32)).astype(x.dtype)


def t5_bucket(dist):
    nf = jnp.maximum(dist, 1).astype(jnp.float32)
    large = MAX_EXACT + (jnp.log(nf / MAX_EXACT) / math.log(MAX_DISTANCE / MAX_EXACT)
                         * (N_BUCKETS - MAX_EXACT)).astype(jnp.int32)
    large = jnp.minimum(large, N_BUCKETS - 1)
    return jnp.where(dist < MAX_EXACT, dist, large)


def dilated_branch(q, k, v, rel_bias, window, dilation):
    B, H, S, hd = q.shape
    d = dilation
    W = window // dilation
    L = S // d
    nb = -(-L // W)
    Lp = nb * W

    def to_blocks(t):
        t = t.reshape(B, H, L, d, hd).transpose(0, 1, 3, 2, 4)
        t = jnp.pad(t, ((0, 0), (0, 0), (0, 0), (0, Lp - L), (0, 0)))
        return t.reshape(B, H, d, nb, W, hd)

    qb, kb, vb = to_blocks(q), to_blocks(k), to_blocks(v)

    def with_prev(t):
        prev = jnp.pad(t[:, :, :, :-1], ((0, 0), (0, 0), (0, 0), (1, 0), (0, 0), (0, 0)))
        return jnp.concatenate([prev, t], axis=-2)

    kk, vv = with_prev(kb), with_prev(vb)
    a = jnp.arange(W)[:, None]
    c = jnp.arange(2 * W)[None, :]
    delta = W + a - c
    blk = jnp.arange(nb)[:, None, None]
    valid = (delta >= 0) & (delta <= W) & (blk * W + c - W >= 0)
    bucket = t5_bucket(jnp.maximum(delta, 0) * d)
    bias = rel_bias[bucket].transpose(2, 0, 1).astype(jnp.float32)

    s = jnp.einsum('bhrnqe,bhrnke->bhrnqk', qb, kk).astype(jnp.float32) * (hd ** -0.5)
    s = s + bias[None, :, None, None]
    s = jnp.where(valid[None, None, None], s, -jnp.inf)
    m = jnp.max(s, axis=-1, keepdims=True)
    p = jnp.exp(s - m)
    den = jnp.sum(p, axis=-1, keepdims=True)
    o = jnp.einsum('bhrnqk,bhrnke->bhrnqe', p, vv.astype(jnp.float32)) / den
    lse = m + jnp.log(den)

    def from_blocks(t):
        e = t.shape[-1]
        t = t.reshape(B, H, d, Lp, e)[:, :, :, :L]
        return t.transpose(0, 1, 3, 2, 4).reshape(B, H, S, e)

    return from_blocks(o), from_blocks(lse)[..., 0]


def dilated_attention(q, k, v, rel_bias):
    outs, lses = [], []
    for window, dilation in DILATED_CONFIGS:
        o, l = dilated_branch(q, k, v, rel_bias, window, dilation)
        outs.append(o)
        lses.append(l)
    wts = jax.nn.softmax(jnp.stack(lses, 0), axis=0)
    return jnp.einsum('gbhs,gbhse->bhse', wts, jnp.stack(outs, 0))


def gla_chunked(q, k, v, log_a):
    B, H, S, dk = q.shape
    dv = v.shape[-1]
    C = GLA_CHUNK
    N = S // C
    q = q.reshape(B, H, N, C, dk)
    k = k.reshape(B, H, N, C, dk)
    v = v.reshape(B, H, N, C, dv)
    b = jnp.cumsum(log_a.reshape(B, H, N, C, dk), axis=-2)
    b_last = b[..., C - 1:, :]
    b_ref = b[..., C // 2 - 1:C // 2, :]
    attn = jnp.einsum('bhnik,bhnjk->bhnij', q * jnp.exp(b - b_ref), k * jnp.exp(b_ref - b))
    causal = jnp.tril(jnp.ones((C, C), dtype=bool))
    attn = jnp.where(causal, attn, 0.0)
    o_intra = jnp.einsum('bhnij,bhnjv->bhniv', attn, v)
    kv = jnp.einsum('bhnck,bhncv->bhnkv', k * jnp.exp(b_last - b), v)
    decay = jnp.exp(b_last[..., 0, :])

    def step(state, inp):
        kv_n, dec_n = inp
        return dec_n[..., None] * state + kv_n, state

    _, states = lax.scan(step, jnp.zeros((B, H, dk, dv), jnp.float32),
                         (jnp.moveaxis(kv, 2, 0), jnp.moveaxis(decay, 2, 0)))
    states = jnp.moveaxis(states, 0, 2)
    o_inter = jnp.einsum('bhnck,bhnkv->bhncv', q * jnp.exp(b), states)
    return (o_intra + o_inter).reshape(B, H, S, dv)


def peer_ffn(xn, w_query, keys1, keys2, u, v):
    B, S, D = xn.shape
    T = B * S
    xt = xn.reshape(T, D)
    q = (xt @ w_query).reshape(T, PEER_HEADS, 2, PEER_QUERY_DIM // 2)
    s1 = jnp.einsum('thc,kc->thk', q[:, :, 0], keys1).astype(jnp.float32)
    s2 = jnp.einsum('thc,kc->thk', q[:, :, 1], keys2).astype(jnp.float32)
    v1, i1 = lax.top_k(s1, PEER_TOPK)
    v2, i2 = lax.top_k(s2, PEER_TOPK)
    cand = (v1[..., :, None] + v2[..., None, :]).reshape(T, PEER_HEADS, PEER_TOPK * PEER_TOPK)
    cand_idx = (i1[..., :, None] * PEER_N_KEYS + i2[..., None, :]).reshape(T, PEER_HEADS, PEER_TOPK * PEER_TOPK)
    top_s, pos = lax.top_k(cand, PEER_TOPK)
    experts = jnp.take_along_axis(cand_idx, pos, axis=-1)
    gates = jax.nn.softmax(top_s, axis=-1).astype(xn.dtype)
    nblk = T // PEER_TOKEN_BLOCK

    def block(args):
        xb, eb, gb = args
        ue = u[eb]
        act = jax.nn.gelu(jnp.einsum('thkd,td->thk', ue, xb), approximate=False)
        return jnp.einsum('thk,thkd->td', gb * act, v[eb])

    out = lax.map(block, (xt.reshape(nblk, PEER_TOKEN_BLOCK, D),
                          experts.reshape(nblk, PEER_TOKEN_BLOCK, PEER_HEADS, PEER_TOPK),
                          gates.reshape(nblk, PEER_TOKEN_BLOCK, PEER_HEADS, PEER_TOPK)))
    return out.reshape(B, S, D)


def split_heads(t, n_heads):
    B, S, E = t.shape
    return t.reshape(B, S, n_heads, E // n_heads).transpose(0, 2, 1, 3)


def merge_heads(t):
    B, H, S, e = t.shape
    return t.transpose(0, 2, 1, 3).reshape(B, S, H * e)


def setup_inputs(seed: int = 0) -> dict:
    key = jax.random.key(seed)
    ks = jax.random.split(key, 16)
    f32 = jnp.float32
    nrm = lambda k, shape, scale: jax.random.normal(k, shape, f32) * scale
    return {
        "x": nrm(ks[0], (BATCH, SEQ, D_MODEL), 1.0),
        "ln1_g": 1.0 + nrm(ks[1], (DEPTH, D_MODEL), 0.02),
        "w_in": nrm(ks[2], (DEPTH, D_MODEL, D_IN), D_MODEL ** -0.5),
        "rel_bias": nrm(ks[3], (N_BUCKETS, N_ATTN_HEADS), 0.1),
        "gla_w_gate2": nrm(ks[4], (DEPTH, GLA_GATE_RANK, D_GLA_K), GLA_GATE_RANK ** -0.5),
        "gla_b_gate": nrm(ks[5], (DEPTH, D_GLA_K), 0.1),
        "gla_norm_g": 1.0 + nrm(ks[6], (DEPTH, N_GLA_HEADS, GLA_DV), 0.02),
        "w_out": nrm(ks[7], (DEPTH, D_MIX, D_MODEL), D_MIX ** -0.5),
        "ln2_g": 1.0 + nrm(ks[8], (DEPTH, D_MODEL), 0.02),
        "peer_w_query": nrm(ks[9], (DEPTH, D_MODEL, PEER_HEADS * PEER_QUERY_DIM), D_MODEL ** -0.5),
        "peer_keys1": nrm(ks[10], (DEPTH, PEER_N_KEYS, PEER_QUERY_DIM // 2), (PEER_QUERY_DIM // 2) ** -0.5),
        "peer_keys2": nrm(ks[11], (DEPTH, PEER_N_KEYS, PEER_QUERY_DIM // 2), (PEER_QUERY_DIM // 2) ** -0.5),
        "peer_u": nrm(ks[12], (DEPTH, PEER_N_EXPERTS, D_MODEL), D_MODEL ** -0.5),
        "peer_v": nrm(ks[13], (DEPTH, PEER_N_EXPERTS, D_MODEL), PEER_HEADS ** -0.5),
        "ln_f_g": 1.0 + nrm(ks[14], (D_MODEL,), 0.02),
    }


def reference(x, ln1_g, w_in, rel_bias, gla_w_gate2, gla_b_gate, gla_norm_g, w_out,
              ln2_g, peer_w_query, peer_keys1, peer_keys2, peer_u, peer_v, ln_f_g):
    h = x
    for l in range(DEPTH):
        xn = rms_norm(h, ln1_g[l])
        proj = xn @ w_in[l]
        bounds = []
        acc = 0
        for w in IN_SPLITS[:-1]:
            acc += w
            bounds.append(acc)
        aq, ak, av, gq, gk, gv, ga, gr = jnp.split(proj, bounds, axis=-1)
        attn_o = dilated_attention(split_heads(aq, N_ATTN_HEADS), split_heads(ak, N_ATTN_HEADS),
                                   split_heads(av, N_ATTN_HEADS), rel_bias)
        attn_o = merge_heads(attn_o).astype(h.dtype)
        gate_logit = (ga @ gla_w_gate2[l] + gla_b_gate[l]).astype(jnp.float32)
        log_a = jax.nn.log_sigmoid(gate_logit) / GLA_GATE_TEMP
        gla_o = gla_chunked(split_heads(gq, N_GLA_HEADS).astype(jnp.float32) * (GLA_DK ** -0.5),
                            split_heads(gk, N_GLA_HEADS).astype(jnp.float32),
                            split_heads(gv, N_GLA_HEADS).astype(jnp.float32),
                            split_heads(log_a, N_GLA_HEADS))
        gla_o = rms_norm(gla_o, gla_norm_g[l][:, None, :])
        gla_o = (merge_heads(gla_o) * jax.nn.silu(gr.astype(jnp.float32))).astype(h.dtype)
        mix = jnp.concatenate([attn_o, gla_o], axis=-1) @ w_out[l]
        h = h + mix
        hn = rms_norm(h, ln2_g[l])
        h = h + peer_ffn(hn, peer_w_query[l], peer_keys1[l], peer_keys2[l], peer_u[l], peer_v[l])
    return rms_norm(h, ln_f_g)
```

```python
import math
from contextlib import ExitStack

import numpy as np
import ml_dtypes

import concourse.bass as bass
import concourse.mybir as mybir
from concourse.bass_utils import run_bass_kernel_spmd

F32 = mybir.dt.float32
BF16 = mybir.dt.bfloat16
I32 = mybir.dt.int32
U32 = mybir.dt.uint32
AF = mybir.ActivationFunctionType
ALU = mybir.AluOpType
AX = mybir.AxisListType

D = 2048
S = 2048
NT = 16
D_IN = 6160
EPS = 1e-6
LTAB = 2560
NEXP = 16384

ENGS = ("pe", "act", "dve", "pool", "sp")


class Sched:
    def __init__(self):
        self.ops = []

    default_cost = 0.12

    def add(self, eng, fn, reads=(), writes=(), dma=None, cost=None):
        self.ops.append(dict(kind="op", eng=eng, fn=fn, reads=tuple(reads), writes=tuple(writes), dma=dma,
                             cost=self.default_cost if cost is None else cost))

    def pe(self, fn, reads=(), writes=()):
        self.add("pe", fn, reads, writes)

    def act(self, fn, reads=(), writes=()):
        self.add("act", fn, reads, writes)

    def dve(self, fn, reads=(), writes=()):
        self.add("dve", fn, reads, writes)

    def pool(self, fn, reads=(), writes=()):
        self.add("pool", fn, reads, writes)

    def dma(self, queue, group, fn, reads=(), writes=()):
        self.add(queue, fn, reads, writes, dma=group)

    def barrier(self):
        self.ops.append(dict(kind="barrier"))

    def analyse(self):
        ops = self.ops
        last_writer = {}
        readers = {}
        eng_pos = {e: 0 for e in ENGS}
        eng_last = {e: None for e in ENGS}
        dma_count = {}
        pending_bar = {e: None for e in ENGS}
        for i, op in enumerate(ops):
            if op["kind"] == "barrier":
                info = dict(eng={e: eng_last[e] for e in ENGS if eng_last[e] is not None}, dma=dict(dma_count))
                for oid in info["eng"].values():
                    ops[oid]["signal"] = True
                for e in ENGS:
                    if pending_bar[e] is None:
                        pending_bar[e] = info
                    else:
                        pending_bar[e] = info
                last_writer.clear()
                readers.clear()
                continue
            e = op["eng"]
            op["pos"] = eng_pos[e]
            eng_pos[e] += 1
            op["signal"] = op.get("signal", False)
            deps = set()
            for r in op["reads"]:
                w = last_writer.get(r)
                if w is not None:
                    deps.add((w, "raw"))
            for wr in op["writes"]:
                w = last_writer.get(wr)
                if w is not None:
                    deps.add((w, "waw"))
                for rd in readers.get(wr, ()):
                    deps.add((rd, "war"))
            waits = []
            best_eng = {}
            best_dma = {}
            for (pid, kind) in deps:
                if pid == i:
                    continue
                p = ops[pid]
                if p["dma"] is not None:
                    if p["dma_idx"] > best_dma.get(p["dma"], 0):
                        best_dma[p["dma"]] = p["dma_idx"]
                    continue
                if p["eng"] == e and op["dma"] is None:
                    if e == "pe":
                        continue
                if pid > best_eng.get(p["eng"], -1):
                    best_eng[p["eng"]] = pid
            for e2, pid in best_eng.items():
                ops[pid]["signal"] = True
                waits.append(("eng", e2, pid))
            for g, c in best_dma.items():
                waits.append(("dma", g, c))
            if pending_bar[e] is not None:
                info = pending_bar[e]
                for e2, oid in info["eng"].items():
                    if not (e2 == e == "pe"):
                        waits.append(("eng", e2, oid))
                for g, c in info["dma"].items():
                    waits.append(("dma", g, c))
                pending_bar[e] = None
            op["waits"] = waits
            if op["dma"] is not None:
                g = op["dma"]
                dma_count[g] = dma_count.get(g, 0) + 1
                op["dma_idx"] = dma_count[g]
            else:
                eng_last[e] = i
            for r in op["reads"]:
                readers.setdefault(r, []).append(i)
            for wr in op["writes"]:
                last_writer[wr] = i
                readers[wr] = []
        self.dma_count = dma_count
        cnt = {e: 0 for e in ENGS}
        for op in ops:
            if op["kind"] != "op" or op["dma"] is not None:
                continue
            if op["signal"]:
                cnt[op["eng"]] += 1
                op["sigval"] = cnt[op["eng"]]
        self.sig_count = cnt

    def emit_engine(self, ename, eng, sems, dsems):
        known = {}
        n = 0
        for op in self.ops:
            if op["kind"] != "op" or op["eng"] != ename:
                continue
            need = {}
            for w in op["waits"]:
                if w[0] == "dma":
                    key = ("dma", w[1])
                    val = 16 * w[2]
                else:
                    key = ("eng", w[1])
                    val = self.ops[w[2]]["sigval"]
                if val > need.get(key, 0):
                    need[key] = val
            for key, val in need.items():
                if known.get(key, 0) >= val:
                    continue
                known[key] = val
                sem = dsems[key[1]] if key[0] == "dma" else sems[key[1]]
                eng.wait_ge(sem, val)
            ins = op["fn"](eng)
            if op["dma"] is not None:
                ins.then_inc(dsems[op["dma"]], 16)
            elif op["signal"]:
                ins.then_inc(sems[ename], 1)
            n += 1
        return known

    def final_waits(self, ename, eng, sems, dsems, known):
        for g, c in self.dma_count.items():
            if known.get(("dma", g), 0) < 16 * c:
                eng.wait_ge(dsems[g], 16 * c)
        for e2, c in self.sig_count.items():
            if e2 != ename and c > 0 and known.get(("eng", e2), 0) < c:
                eng.wait_ge(sems[e2], c)


def _t5_bucket(dist):
    dist = np.asarray(dist, dtype=np.int64)
    nf = np.maximum(dist, 1).astype(np.float32)
    large = 16 + (np.log(nf / np.float32(16)) / np.float32(math.log(2048 / 16)) * np.float32(16)).astype(np.int32)
    large = np.minimum(large, 31)
    return np.where(dist < 16, dist, large)


def _consts():
    c = {}
    c["c_ident"] = np.eye(128, dtype=np.float32).astype(ml_dtypes.bfloat16)
    y = np.arange(LTAB)
    dist = y - 511
    mult = ((dist >= 0) & (dist <= 128)).astype(np.int64) \
        + ((dist >= 0) & (dist <= 512) & (dist % 4 == 0)).astype(np.int64) \
        + ((dist >= 0) & (dist <= 2048) & (dist % 16 == 0)).astype(np.int64)
    bucket = _t5_bucket(np.maximum(dist, 0))
    oh = np.zeros((32, LTAB), dtype=np.float32)
    valid = mult > 0
    oh[bucket[valid], y[valid]] = mult[valid].astype(np.float32)
    c["c_onehot"] = oh.astype(ml_dtypes.bfloat16)
    t = np.arange(128)
    incl = (t[:, None] <= t[None, :]).astype(np.float32)
    ref = (t[:, None] <= 63).astype(np.float32)
    s = np.float32(-1.0 / 16.0)
    g = np.zeros((128, 640), dtype=np.float32)
    g[:, 0:128] = s * (incl - ref)
    g[:, 128:256] = s * incl
    g[:, 256] = s
    g[:, 384:512] = s * (t[:, None] > t[None, :]).astype(np.float32)
    g[:, 512:640] = (t[:, None] <= t[None, :]).astype(np.float32)
    c["c_gla"] = g
    c["c_iota16"] = np.tile(np.arange(16, dtype=np.float32)[None, :], (128, 1))
    return c


class Arena:
    def __init__(self, t, nwords):
        self.t = t
        self.n = nwords
        self.off = 0
        self.peak = 0

    def f32(self, n):
        assert self.off + n <= self.n, f"arena overflow {self.off}+{n}>{self.n}"
        ap = self.t[:, self.off:self.off + n]
        self.off += n
        self.peak = max(self.peak, self.off)
        return ap

    def bf16(self, n):
        w = (n + 1) // 2
        return self.f32(w).bitcast(BF16)[:, 0:n]

    def i32(self, n):
        return self.f32(n).bitcast(I32)

    def u32(self, n):
        return self.f32(n).bitcast(U32)

    def mark(self):
        return self.off

    def release(self, m):
        self.off = m


def v3(ap, b):
    return ap.rearrange("p (a b) -> p a b", b=b)


class Builder:
    def __init__(self, taps=(), stop_after=None, peer_tiles=NT):
        self.peer_tiles = peer_tiles
        self.taps = set(taps)
        self.stop_after = stop_after
        self.S = Sched()
        self.nc = bass.Bass("TRN2", target_bir_lowering=False)
        self.es = ExitStack()
        self.tap_names = []
        self._bank_rr = 0

    def mm(self, out, lhsT, rhs, start, stop, reads, writes):
        self.S.pe(lambda e: e.matmul(out, lhsT, rhs, start=start, stop=stop), reads, writes)

    def actf(self, out, in_, func, reads, writes, bias=None, scale=None, accum_out=None, eng="act"):
        kw = {}
        if bias is not None:
            kw["bias"] = bias
        if scale is not None:
            kw["scale"] = scale
        if accum_out is not None:
            kw["accum_out"] = accum_out
        self.S.act(lambda e: e.activation(out, in_, func, **kw), reads, writes)

    def tt(self, out, in0, in1, op, reads, writes, eng="dve"):
        self.S.add(eng, lambda e: e.tensor_tensor(out, in0, in1, op), reads, writes)

    def ts(self, out, in0, s1, s2, op0, op1, reads, writes, eng="dve", accum_out=None):
        if op1 is None:
            self.S.add(eng, lambda e: e.tensor_scalar(out, in0, s1, None, op0), reads, writes)
        elif accum_out is None:
            self.S.add(eng, lambda e: e.tensor_scalar(out, in0, s1, s2, op0, op1), reads, writes)
        else:
            self.S.add(eng, lambda e: e.tensor_scalar(out, in0, s1, s2, op0, op1, accum_out), reads, writes)

    def stt(self, out, in0, scalar, in1, op0, op1, reads, writes, eng="dve", accum_out=None):
        if accum_out is None:
            self.S.add(eng, lambda e: e.scalar_tensor_tensor(out, in0, scalar, in1, op0, op1), reads, writes)
        else:
            self.S.add(eng, lambda e: e.scalar_tensor_tensor(out, in0, scalar, in1, op0, op1, accum_out), reads, writes)

    def cp(self, out, in_, reads, writes, eng="dve"):
        if eng == "act":
            self.S.act(lambda e: e.copy(out, in_), reads, writes)
        else:
            self.S.add(eng, lambda e: e.tensor_copy(out, in_), reads, writes)

    def recip(self, out, in_, reads, writes):
        self.S.dve(lambda e: e.reciprocal(out, in_), reads, writes)

    def memset(self, ap, val, writes, eng="dve"):
        self.S.add(eng, lambda e: e.memset(ap, val), (), writes)

    def dma(self, queue, group, out, in_, reads, writes, **kw):
        self.S.dma(queue, group, lambda e: e.dma_start(out=out, in_=in_, **kw), reads, writes)

    def tap(self, name, src, shape, dtype, reads):
        if name not in self.taps:
            return
        t = self.nc.dram_tensor("tap_" + name, list(shape), dtype, kind="ExternalOutput")
        self.tap_names.append("tap_" + name)
        self.dma("sp", "tap_" + name, t.ap(), src, reads, [("tapd", name)])

    def bank(self):
        b = self._bank_rr % 8
        self._bank_rr += 1
        return b

    def declare(self):
        nc = self.nc
        di = lambda n, s, d=F32: nc.dram_tensor(n, list(s), d, kind="ExternalInput")
        self.x_d = di("x", [S, D])
        self.ln1_d = di("ln1_g", [1, D])
        self.w_in_d = di("w_in", [D, D_IN])
        self.rb_d = di("rel_bias", [32, 16])
        self.wg2_d = di("gla_w_gate2", [16, 512])
        self.bg_d = di("gla_b_gate", [1, 512])
        self.gn_d = di("gla_norm_g", [4, 256])
        self.w_out_d = di("w_out", [D, D])
        self.ln2_d = di("ln2_g", [1, D])
        self.wq_d = di("peer_w_query", [D, D])
        self.k1_d = di("peer_keys1", [128, 128])
        self.k2_d = di("peer_keys2", [128, 128])
        self.u_d = di("peer_u", [NEXP, D])
        self.v_d = di("peer_v", [NEXP, D])
        self.lnf_d = di("ln_f_g", [1, D])
        self.c_ident_d = di("c_ident", [128, 128], BF16)
        self.c_onehot_d = di("c_onehot", [32, LTAB], BF16)
        self.c_gla_d = di("c_gla", [128, 640])
        self.c_iota16_d = di("c_iota16", [128, 16])
        self.out_d = nc.dram_tensor("out", [S, D], F32, kind="ExternalOutput")
        self.ebscr_d = nc.dram_tensor("ebscr", [16, 128, LTAB + 1], BF16, kind="Internal")
        self.mixT_d = nc.dram_tensor("mixT", [D, S], BF16, kind="Internal")
        self.h1_d = nc.dram_tensor("h1", [S, D], F32, kind="Internal")
        self.uv16_d = nc.dram_tensor("uv16", [NEXP, 2 * D], BF16, kind="Internal")
        self._tc_pending = []
        for r0 in range(0, NEXP, 512):
            self._tc_pending.append((self.u_d, 0, "tcu", "tabu", r0))
            self._tc_pending.append((self.v_d, D, "tcv", "tabv", r0))

        es = self.es
        sb = lambda n, s, d: es.enter_context(nc.sbuf_tensor(n, list(s), d))
        self.ident = sb("ident", [128, 128], BF16)
        self.gbc = sb("gbc", [128, D], F32)
        self.small = sb("small", [128, 256], F32)
        self.ones_f = sb("ones_f", [128, 128], F32)
        self.ones_b = sb("ones_b", [128, 128], BF16)
        ARENA_WORDS = 49 * 1024
        self.arena_t = sb("arena", [128, ARENA_WORDS], F32)
        self.A = Arena(self.arena_t, ARENA_WORDS)
        self.ps = [es.enter_context(nc.psum_tensor(f"ps{b}", [128, 512], F32)) for b in range(8)]

    def phase0(self):
        A = self.A
        m0 = A.mark()
        self.dma("sp", "c_ident", self.ident[:], self.c_ident_d.ap(), [], [("ident",)])
        self.dma("sp", "gbc", self.gbc[:], self.ln1_d[0:1, :].partition_broadcast(128), [], [("gbc",)])
        self.memset(self.ones_f[:], 1.0, [("ones_f",)])
        self.memset(self.ones_b[:], 1.0, [("ones_b",)])
        self.memset(self.small[:], 0.0, [("small",)])
        self.eps = self.small[:, 255:256]
        self.memset(self.eps, EPS, [("small",)])
        rb = A.f32(16)
        erb = A.bf16(16)
        oh = A.bf16(LTAB)
        rbh = A.bf16(128)
        reps = [A.bf16(LTAB), A.bf16(LTAB)]
        self.dma("sp", "c_rb", rb[0:32, :], self.rb_d.ap(), [], [("rb",)])
        self.dma("sp", "c_oh", oh[0:32, :], self.c_onehot_d.ap(), [], [("oh",)])
        self.actf(erb[0:32, :], rb[0:32, :], AF.Exp, [("rb",)], [("erb",)])
        for h in range(16):
            rep = reps[h % 2]
            self.cp(rbh[0:32, :], erb[0:32, h:h + 1].to_broadcast([32, 128]), [("erb",)], [("rbh",)])
            for ch in range(LTAB // 512):
                b = self.bank()
                sl = slice(ch * 512, (ch + 1) * 512)
                self.mm(self.ps[b][:, :], rbh[0:32, :], oh[0:32, sl], True, True, [("rbh",), ("oh",)], [("ps", b)])
                self.cp(rep[:, sl], self.ps[b][:, :], [("ps", b)], [("rep", h % 2)], eng=("act" if ch % 2 == 0 else "dve"))
            self.dma("sp", f"rep{h % 2}", self.ebscr_d[h, :, 0:LTAB], rep, [("rep", h % 2)], [("ebscr", h)])
        self.S.barrier()
        A.release(m0)

    def phase1(self):
        A = self.A
        self.xnT = v3(A.bf16(16 * S), S)
        self.wbuf = [v3(A.bf16(16 * 512), 512), v3(A.bf16(16 * 512), 512)]
        self.wissue()
        self.wissue()
        m0 = A.mark()
        NX = 4
        xin = [A.f32(D) for _ in range(NX)]
        xnb = [A.bf16(D), A.bf16(D)]
        junk = A.bf16(D)
        ss = self.small[:, 0:16]
        sd = self.small[:, 16:32]
        rstd = self.small[:, 32:48]

        def load(i):
            s = i % NX
            self.dma("sp", f"xin{s}", xin[s], self.x_d[i * 128:(i + 1) * 128, :], [], [("xin", s)])

        def stats(i):
            s = i % NX
            self.actf(junk, xin[s], AF.Square, [("xin", s)], [("junk",), ("ss", i)], accum_out=ss[:, i:i + 1])
            self.actf(sd[:, i:i + 1], ss[:, i:i + 1], AF.Ln, [("ss", i), ("small",)], [("sd", i)],
                      bias=self.eps, scale=1.0 / D)
            self.actf(rstd[:, i:i + 1], sd[:, i:i + 1], AF.Exp, [("sd", i)], [("rstd", i)], scale=-0.5)

        def norm_t(i):
            s = i % NX
            xb = xnb[i % 2]
            self.stt(xb, xin[s], rstd[:, i:i + 1], self.gbc[:], ALU.mult, ALU.mult,
                     [("xin", s), ("rstd", i), ("gbc",)], [("xnb", i % 2)])
            for g4 in range(4):
                b = self.bank()
                for j in range(4):
                    kc = g4 * 4 + j
                    self.mm(self.ps[b][:, j * 128:(j + 1) * 128], xb[:, kc * 128:(kc + 1) * 128], self.ident[:],
                            True, True, [("xnb", i % 2), ("ident",)], [("ps", b)])
                self.cp(self.xnT[:, g4 * 4:(g4 + 1) * 4, i * 128:(i + 1) * 128], v3(self.ps[b][:, :], 128),
                        [("ps", b)], [("xnT", i)], eng=("act" if g4 % 2 == 0 else "dve"))

        for i in range(min(3, NT)):
            load(i)
        stats(0)
        for i in range(NT):
            if i + 3 < NT:
                load(i + 3)
            if i + 1 < NT:
                stats(i + 1)
            norm_t(i)
        self.tap("small", self.small[:, 0:48], [128, 48], F32, [("rstd", i) for i in range(NT)])
        A.release(m0)
        self.S.barrier()

    def wissue(self):
        if self._wnext >= len(self.wplan):
            return
        w_d, col0, ncols = self.wplan[self._wnext]
        self.wload(w_d, col0, ncols, self._wnext % 2)
        self._wnext += 1

    def wtake(self, col0, ncols):
        k = self._wtaken
        assert self.wplan[k][1] == col0 and self.wplan[k][2] == ncols, (self.wplan[k], col0, ncols)
        self._wtaken += 1
        return k % 2

    def wload(self, w_d, col0, ncols, slot):
        wb = self.wbuf[slot]
        for q in range(4):
            src = w_d[q * 512:(q + 1) * 512, col0:col0 + ncols].rearrange("(kc p) n -> p kc n", p=128)
            self.dma("pool", f"w{slot}", wb[:, q * 4:(q + 1) * 4, 0:ncols], src, [], [("wbuf", slot)])

    def proj_fm(self, slot, ncols, evac):
        wb = self.wbuf[slot]
        for m in range((ncols + 127) // 128):
            mw = min(128, ncols - m * 128)
            for tc in range(4):
                b = self.bank()
                for kc in range(16):
                    self.mm(self.ps[b][0:mw, :], wb[:, kc, m * 128:m * 128 + mw], self.xnT[:, kc, tc * 512:(tc + 1) * 512],
                            kc == 0, kc == 15, [("wbuf", slot)] + [("xnT", 4 * tc + j) for j in range(4)], [("ps", b)])
                evac(m, tc, self.ps[b], b)

    def proj_tm(self, slot, ncols, evac):
        wb = self.wbuf[slot]
        for i in range(NT):
            b = self.bank()
            for kc in range(16):
                self.mm(self.ps[b][:, 0:ncols], self.xnT[:, kc, i * 128:(i + 1) * 128], wb[:, kc, 0:ncols],
                        kc == 0, kc == 15, [("wbuf", slot), ("xnT", i)], [("ps", b)])
            evac(i, self.ps[b], b)

    def attention_half(self, half):
        A = self.A
        m0 = A.mark()
        aqT = v3(A.bf16(4 * S), S)
        akT = v3(A.bf16(4 * S), S)
        av = A.bf16(16 * 8 * 65).rearrange("p (i h e) -> p i h e", h=8, e=65)
        EB = [A.bf16(2432) for _ in range(4)]
        E = [A.bf16(512) for _ in range(4)]
        P = [A.bf16(512) for _ in range(4)]
        ostage = [A.bf16(S), A.bf16(S)]
        rc = A.f32(512)
        bcs = A.f32(512)
        hs = f"h{half}"
        for nm, col0, dst in (("aq", half * 512, aqT), ("ak", 1024 + half * 512, akT)):
            slot = self.wtake(col0, 512)

            def evac(m, tc, ps, b, dst=dst, nm=nm):
                self.cp(dst[:, m, tc * 512:(tc + 1) * 512], ps[:, :], [("ps", b)], [(nm, m)],
                        eng=("act" if (m + tc) % 2 == 0 else "dve"))
            self.proj_fm(slot, 512, evac)
            self.wissue()
        slot = self.wtake(2048 + half * 512, 512)
        self.memset(av[:, :, :, 64:65], 1.0, [("av", i) for i in range(NT)])

        def evac_v(i, ps, b):
            self.cp(av[:, i, :, 0:64], v3(ps[:, :], 64), [("ps", b)], [("av", i)], eng=("act" if i % 2 == 0 else "dve"))
        self.proj_tm(slot, 512, evac_v)
        self.wissue()
        if half == 0:
            self.tap("aqT0", aqT[:, 0, :], [128, S], BF16, [("aq", 0)])
            self.tap("akT0", akT[:, 0, :], [128, S], BF16, [("ak", 0)])
            self.tap("av0", av[:, 0, :, :], [128, 8, 65], BF16, [("av", 0)])

        steps = []
        for pr in range(4):
            for c in range(4):
                for kb in range(4 * c + 4):
                    steps.append((pr, c, kb))
        SB = [0, 1, 2, 3]
        OBP = [4, 5, 7]
        BCB = 6
        ob_of = {}

        def load_eb(pr):
            for h2 in range(2):
                hh = pr * 2 + h2
                h = half * 8 + hh
                src = bass.AP(self.ebscr_d, h * 128 * (LTAB + 1) + 127, [[LTAB, 128], [1, 2432]])
                self.dma("sp", f"EB{hh % 4}", EB[hh % 4], src, [("ebscr", h)], [("EB", hh % 4)])

        def emit_S(n):
            pr, c, kb = steps[n]
            for h2 in range(2):
                hh = pr * 2 + h2
                base = h2 * 64
                b = SB[(2 * n + h2) % 4]
                if c == 0 and kb == 0 and h2 == 0 and pr + 1 < 4:
                    load_eb(pr + 1)
                self.mm(self.ps[b][:, :], akT[base:base + 64, pr, kb * 128:(kb + 1) * 128],
                        aqT[base:base + 64, pr, c * 512:(c + 1) * 512], True, True,
                        [("ak", pr), ("aq", pr)], [("ps", b)])

        def emit_rest(n):
            pr, c, kb = steps[n]
            nkb = 4 * c + 4
            for h2 in range(2):
                hh = pr * 2 + h2
                b = SB[(2 * n + h2) % 4]
                es = (2 * n + h2) % 4
                if kb == 0:
                    ob_of[(hh, c)] = OBP[len(ob_of) % 3]
                ob = ob_of[(hh, c)]
                self.actf(E[es], self.ps[b][:, :], AF.Exp, [("ps", b)], [("E", es)], scale=0.125)
                s0 = 512 * c - 128 * kb + 384
                self.tt(P[es], E[es], EB[hh % 4][:, s0:s0 + 512], ALU.mult, [("E", es), ("EB", hh % 4)], [("P", es)])
                self.mm(self.ps[ob][0:65, :], av[:, kb, hh, :], P[es], kb == 0, kb == nkb - 1,
                        [("av", kb), ("P", es)], [("ps", ob)])
            if kb == nkb - 1:
                for h2 in range(2):
                    hh = pr * 2 + h2
                    ob = ob_of[(hh, c)]
                    osl = hh % 2
                    self.actf(rc[64:65, :], self.ps[ob][64:65, :], AF.Ln, [("ps", ob)], [("rc",)])
                    self.actf(rc[64:65, :], rc[64:65, :], AF.Exp, [("rc",)], [("rc",)], scale=-1.0)
                    self.mm(self.ps[BCB][0:64, :], self.ones_f[64:65, 0:64], rc[64:65, :], True, True,
                            [("rc",), ("ones_f",)], [("ps", BCB)])
                    self.cp(bcs[0:64, :], self.ps[BCB][0:64, :], [("ps", BCB)], [("bcs",)], eng="act")
                    self.tt(ostage[osl][0:64, c * 512:(c + 1) * 512], self.ps[ob][0:64, :], bcs[0:64, :], ALU.mult,
                            [("ps", ob), ("bcs",)], [("ostage", osl)])
                    if c == 3:
                        h = half * 8 + hh
                        self.dma("sp", f"ost{osl}", self.mixT_d[h * 64:(h + 1) * 64, :], ostage[osl][0:64, :],
                                 [("ostage", osl)], [("mixT", h)])

        load_eb(0)
        self.table_cast(26)
        LOOK = 1
        for n in range(min(LOOK, len(steps))):
            emit_S(n)
        for n in range(len(steps)):
            if n + LOOK < len(steps):
                emit_S(n + LOOK)
            emit_rest(n)
        A.release(m0)
        self.S.barrier()


    def gla_setup(self):
        A = self.A
        self.gaT = A.f32(S)
        self.w2 = A.f32(512)
        self.bg = A.f32(512)
        self.gn = A.f32(8)
        self.cg = A.f32(640)
        self.dma("sp", "c_w2", self.w2[0:16, :], self.wg2_d.ap(), [], [("w2",)])
        self.dma("sp", "c_bg", self.bg[0:1, :], self.bg_d.ap(), [], [("bg",)])
        self.dma("sp", "c_gn", self.gn, self.gn_d.ap().rearrange("h (c p) -> p h c", p=128), [], [("gn",)],
                 allow_slow_non_contiguous=True)
        self.dma("sp", "c_cg", self.cg, self.c_gla_d.ap(), [], [("cg",)])
        slot = self.wtake(5120, 16)

        def evac(m, tc, ps, b):
            self.cp(self.gaT[0:16, tc * 512:(tc + 1) * 512], ps[0:16, :], [("ps", b)], [("gaT",)], eng="dve")
        self.proj_fm(slot, 16, evac)
        self.wissue()

    def gla_pair(self, hp):
        A = self.A
        m0 = A.mark()
        gqT = v3(A.bf16(2 * S), S)
        gkT = v3(A.bf16(2 * S), S)
        QtT = v3(A.bf16(2 * S), S)
        gkt = v3(A.bf16(16 * 256), 256)
        gvt = v3(A.bf16(16 * 512), 512)
        grT = v3(A.bf16(4 * S), S)
        ostage = grT
        dec = v3(A.f32(32), 2)
        la = [A.f32(256), A.f32(256)]
        ez = A.f32(256)
        e1 = A.f32(256)
        e1i = A.f32(256)
        e4 = A.f32(256)
        e3 = A.f32(258)
        Sf = v3(A.f32(512), 256)
        Sb = v3(A.bf16(512), 256)
        am = [A.bf16(128), A.bf16(128)]
        sq = A.f32(256)
        sdn = A.f32(128)
        rs = A.f32(128)
        t1 = A.f32(128)
        cg = self.cg
        W = self.w_in_d
        slot = self.wtake(3072 + hp * 256, 256)

        def ev_q(m, tc, ps, b):
            self.actf(gqT[:, m, tc * 512:(tc + 1) * 512], ps[:, :], AF.Copy, [("ps", b)], [("gqT", m)], scale=128.0 ** -0.5)
        self.proj_fm(slot, 256, ev_q)
        self.wissue()
        slot = self.wtake(3584 + hp * 256, 256)

        def ev_k(m, tc, ps, b):
            self.cp(gkT[:, m, tc * 512:(tc + 1) * 512], ps[:, :], [("ps", b)], [("gkT", m)],
                    eng=("act" if tc % 2 == 0 else "dve"))
        self.proj_fm(slot, 256, ev_k)

        def ev_kt(i, ps, b):
            self.cp(gkt[:, i, :], ps[:, 0:256], [("ps", b)], [("gkt", i)], eng=("act" if i % 2 == 0 else "dve"))
        self.proj_tm(slot, 256, ev_kt)
        self.wissue()
        slot = self.wtake(4096 + hp * 512, 512)

        def ev_v(i, ps, b):
            self.cp(gvt[:, i, :], ps[:, :], [("ps", b)], [("gvt", i)], eng=("act" if i % 2 == 0 else "dve"))
        self.proj_tm(slot, 512, ev_v)
        self.wissue()
        slot = self.wtake(5136 + hp * 512, 512)

        def ev_r(m, tc, ps, b):
            self.actf(grT[:, m, tc * 512:(tc + 1) * 512], ps[:, :], AF.Silu, [("ps", b)], [("grT", m)])
        self.proj_fm(slot, 512, ev_r)
        self.wissue()

        ZB, BA, BB, PA, PO, PK, PN = 0, 1, 2, 3, (4, 5), 6, 7
        ps = self.ps
        for i in range(NT):
            tl = slice(i * 128, (i + 1) * 128)
            l = la[i % 2]
            lr = ("la", i % 2)
            self.mm(ps[ZB][:, 0:256], self.gaT[0:16, tl], self.w2[0:16, hp * 256:(hp + 1) * 256], True, False,
                    [("gaT",), ("w2",)], [("ps", ZB)])
            self.mm(ps[ZB][:, 0:256], self.ones_f[0:1, 0:128], self.bg[0:1, hp * 256:(hp + 1) * 256], False, True,
                    [("ones_f",), ("bg",)], [("ps", ZB)])
            self.actf(ez, ps[ZB][:, 0:256], AF.Exp, [("ps", ZB)], [("ez",)], scale=-1.0)
            self.actf(l, ez, AF.Ln, [("ez",), ("ones_f",)], [lr], bias=self.ones_f[:, 0:1])
            for h2 in range(2):
                self.mm(ps[BA][:, h2 * 128:(h2 + 1) * 128], l[:, h2 * 128:(h2 + 1) * 128], cg[:, 0:128], True, True,
                        [lr, ("cg",)], [("ps", BA)])
                self.mm(ps[BB][:, h2 * 129:(h2 + 1) * 129], l[:, h2 * 128:(h2 + 1) * 128], cg[:, 128:257], True, True,
                        [lr, ("cg",)], [("ps", BB)])
            self.mm(ps[BA][:, 256:512], cg[:, 384:512], l, True, True, [lr, ("cg",)], [("ps", BA)])
            self.actf(e1, ps[BA][:, 0:256], AF.Exp, [("ps", BA)], [("e1",)])
            self.actf(e1i, ps[BA][:, 0:256], AF.Exp, [("ps", BA)], [("e1i",)], scale=-1.0)
            self.actf(e4, ps[BA][:, 256:512], AF.Exp, [("ps", BA)], [("e4",)])
            self.actf(e3, ps[BB][:, 0:258], AF.Exp, [("ps", BB)], [("e3",)])
            self.tt(QtT[:, :, tl], gqT[:, :, tl], v3(e1, 128), ALU.mult, [("gqT", 0), ("gqT", 1), ("e1",)], [("QtT", i)])
            self.tt(gkT[:, :, tl], gkT[:, :, tl], v3(e1i, 128), ALU.mult, [("gkT", 0), ("gkT", 1), ("e1i",)], [("KtT", i)])
            self.tt(gqT[:, :, tl], gqT[:, :, tl], v3(e3, 129)[:, :, 0:128], ALU.mult, [("gqT", 0), ("gqT", 1), ("e3",), ("QtT", i)],
                    [("QhT", i)])
            self.tt(gkt[:, i, :], gkt[:, i, :], e4, ALU.mult, [("gkt", i), ("e4",)], [("Kh", i)])
            self.cp(dec[:, i, :], v3(e3, 129)[:, :, 128], [("e3",)], [("dec", i)], eng="dve")
        self.table_cast(6)
        PA2 = (0, 1)
        PK2 = (2, 3)
        PN = 3
        POB = ((4, 5), (6, 7))

        def rec_A(i):
            tl = slice(i * 128, (i + 1) * 128)
            for h2 in range(2):
                self.mm(ps[PA2[h2]][:, 0:128], gkT[:, h2, tl], QtT[:, h2, tl], True, True, [("KtT", i), ("QtT", i)],
                        [("ps", PA2[h2])])
            for h2 in range(2):
                self.tt(am[h2], ps[PA2[h2]][:, 0:128], cg[:, 512:640], ALU.mult, [("ps", PA2[h2]), ("cg",)], [("am", h2)])
            for h2 in range(2):
                po = POB[h2][i % 2]
                if i < NT - 1:
                    self.mm(ps[PK2[h2]][:, 0:256], gkt[:, i, h2 * 128:(h2 + 1) * 128], gvt[:, i, h2 * 256:(h2 + 1) * 256],
                            True, True, [("Kh", i), ("gvt", i)], [("ps", PK2[h2])])
                for vc in range(2):
                    osl = ps[po][:, vc * 128:(vc + 1) * 128]
                    if i > 0:
                        self.mm(osl, Sb[:, h2, vc * 128:(vc + 1) * 128], gqT[:, h2, tl], True, False,
                                [("Sb", h2), ("QhT", i)], [("ps", po)])
                    self.mm(osl, gvt[:, i, h2 * 256 + vc * 128:h2 * 256 + (vc + 1) * 128], am[h2], i == 0, True,
                            [("gvt", i), ("am", h2)], [("ps", po)])
            if i < NT - 1:
                for h2 in range(2):
                    if i == 0:
                        self.cp(Sf[:, h2, :], ps[PK2[h2]][:, 0:256], [("ps", PK2[h2])], [("Sf", h2)], eng="dve")
                    else:
                        self.stt(Sf[:, h2, :], Sf[:, h2, :], dec[:, i, h2:h2 + 1], ps[PK2[h2]][:, 0:256], ALU.mult, ALU.add,
                                 [("Sf", h2), ("dec", i), ("ps", PK2[h2])], [("Sf", h2)])
                    self.cp(Sb[:, h2, :], Sf[:, h2, :], [("Sf", h2)], [("Sb", h2)], eng="act")

        def rec_N(i):
            tl = slice(i * 128, (i + 1) * 128)
            for h2 in range(2):
                po = POB[h2][i % 2]
                self.actf(sq, ps[po][:, 0:256], AF.Square, [("ps", po)], [("sq",)])
                self.mm(ps[PN][:, 256:384], self.ones_f[:, 0:128], sq[:, 0:128], True, False, [("ones_f",), ("sq",)], [("psn",)])
                self.mm(ps[PN][:, 256:384], self.ones_f[:, 0:128], sq[:, 128:256], False, True, [("ones_f",), ("sq",)], [("psn",)])
                self.actf(sdn, ps[PN][:, 256:384], AF.Ln, [("psn",), ("small",)], [("sdn",)], bias=self.eps, scale=1.0 / 256)
                self.actf(rs, sdn, AF.Exp, [("sdn",)], [("rs",)], scale=-0.5)
                for vc in range(2):
                    k = h2 * 2 + vc
                    self.stt(t1, ps[po][:, vc * 128:(vc + 1) * 128], self.gn[:, k + hp * 4:k + hp * 4 + 1], rs, ALU.mult, ALU.mult,
                             [("ps", po), ("gn",), ("rs",)], [("t1",)])
                    self.tt(ostage[:, k, tl], t1, grT[:, k, tl], ALU.mult, [("t1",), ("grT", k)], [("gost", k)])

        for i in range(NT):
            rec_A(i)
            if i >= 1:
                rec_N(i - 1)
        rec_N(NT - 1)
        r0 = 1024 + hp * 512
        self.dma("sp", "gost", self.mixT_d[r0:r0 + 512, :].rearrange("(k p) t -> p k t", p=128), ostage,
                 [("gost", k) for k in range(4)], [("mixT", 16 + hp)])
        A.release(m0)
        self.S.barrier()


    def phase5(self):
        A = self.A
        m0 = A.mark()
        mixS = self.xnT
        wo = v3(A.bf16(16 * D), D)
        xin = [A.f32(D), A.f32(D)]
        h1s = [A.f32(D), A.f32(D)]
        for q in range(4):
            self.dma("sp", f"mixS{q}", mixS[:, q * 4:(q + 1) * 4, :],
                     self.mixT_d[q * 512:(q + 1) * 512, :].rearrange("(kc p) t -> p kc t", p=128),
                     [("mixT", h) for h in range(18)], [("mixS", q)])
            self.dma("pool", f"wo{q}", wo[:, q * 4:(q + 1) * 4, :],
                     self.w_out_d[q * 512:(q + 1) * 512, :].rearrange("(kc p) n -> p kc n", p=128), [], [("wo", q)])
        for i in range(NT):
            s = i % 2
            self.dma("sp", f"xin{s}", xin[s], self.x_d[i * 128:(i + 1) * 128, :], [], [("xin", s)])
            for n in range(4):
                b = self.bank()
                for kc in range(16):
                    self.mm(self.ps[b][:, :], mixS[:, kc, i * 128:(i + 1) * 128], wo[:, kc, n * 512:(n + 1) * 512],
                            kc == 0, kc == 15, [("mixS", kc // 4), ("wo", kc // 4)], [("ps", b)])
                self.tt(h1s[s][:, n * 512:(n + 1) * 512], self.ps[b][:, :], xin[s][:, n * 512:(n + 1) * 512], ALU.add,
                        [("ps", b), ("xin", s)], [("h1s", s)])
            self.dma("sp", f"h1s{s}", self.h1_d[i * 128:(i + 1) * 128, :], h1s[s], [("h1s", s)], [("h1d", i)])
        A.release(m0)
        self.S.barrier()

    def bcap(self, ap, dims, off=0):
        return bass.AP(ap.tensor, ap.offset + off, [list(ap.ap[0])] + [list(d) for d in dims])

    def top16(self, vout, iout, src, tmp, rsrc, rv, ri, rtmp):
        self.S.dve(lambda e: e.max(out=vout[:, 0:8], in_=src), [rsrc], [rv])
        self.S.dve(lambda e: e.max_index(out=iout[:, 0:8], in_max=vout[:, 0:8], in_values=src), [rsrc, rv], [ri])
        self.S.dve(lambda e: e.match_replace(out=tmp, in_to_replace=vout[:, 0:8], in_values=src, imm_value=-1e30),
                   [rsrc, rv], [rtmp])
        self.S.dve(lambda e: e.max(out=vout[:, 8:16], in_=tmp), [rtmp], [rv])
        self.S.dve(lambda e: e.max_index(out=iout[:, 8:16], in_max=vout[:, 8:16], in_values=tmp), [rtmp, rv], [ri])

    def splice_plan(self, pops, nslots):
        plan = {}
        if not pops:
            return plan
        for cap in (0.2, 0.25, 0.3, 0.4, 0.5, 0.7, 1.0, 1.5, 3.0, 100.0):
            plan = {}
            lastw = {}
            budget = {}
            cur = 0
            for op in pops:
                eng = op["eng"]
                sl = cur
                for r in op["reads"] + op["writes"]:
                    w = lastw.get(r)
                    if w is not None and w[1] != eng and w[0] >= sl:
                        sl = w[0] + 1
                c = op.get("cost", 0.12)
                if eng in ("dve", "act"):
                    while budget.get((sl, eng), 0.0) + c > cap and budget.get((sl, eng), 0.0) > 0:
                        sl += 1
                    budget[(sl, eng)] = budget.get((sl, eng), 0.0) + c
                cur = sl
                for r in op["writes"]:
                    lastw[r] = (sl, eng)
                plan.setdefault(sl, []).append(op)
            if cur < nslots:
                break
        if cur >= 128:
            merged = {}
            for sl, ops in sorted(plan.items()):
                merged.setdefault(min(sl, 127), []).extend(ops)
            plan = merged
        self.last_plan_len = cur
        return plan

    def capture(self, fn):
        saved = self.S.ops
        self.S.ops = []
        fn()
        got = self.S.ops
        self.S.ops = saved
        return got

    def table_cast(self, n):
        for _ in range(n):
            if not self._tc_pending:
                return
            src, c0, grp, nm, r0 = self._tc_pending.pop(0)
            self.dma("pool", grp, self.uv16_d[r0:r0 + 512, c0:c0 + D], src[r0:r0 + 512, :], [], [(nm,)])

    def peer(self):
        A = self.A
        NB = 7
        wq = v3(A.bf16(16 * D), D)
        k1T = A.f32(128)
        k2T = A.f32(128)
        kT = [k1T, k2T]
        identf = A.f32(128)
        gfc = A.f32(D)
        iota = A.f32(16)
        h1b = [A.f32(D) for _ in range(2)]
        hnb = [A.bf16(D) for _ in range(2)]
        junk = A.bf16(D)
        hnT = v3(A.bf16(16 * 128), 128)
        scrA = A.f32(2048)
        scrB = A.f32(2048)
        tmpm = A.f32(256)
        v16 = A.f32(256)
        i16 = A.u32(256)
        i16f = A.f32(256)
        tops = A.f32(128)
        pos = A.u32(128)
        pab = A.u32(128)
        paf = A.f32(128)
        pbf = A.f32(128)
        i1s = A.f32(128)
        i2s = A.f32(128)
        ef = A.f32(128)
        gs = A.f32(8)
        rg = A.f32(8)
        eidx = [A.i32(128) for _ in range(2)]
        gates = [A.f32(128) for _ in range(2)]
        actp = [A.f32(128) for _ in range(2)]
        gl = A.f32(128)
        wv = A.f32(128)
        dg = [A.bf16(128) for _ in range(4)]
        gb = [A.bf16(2 * D) for _ in range(NB)]
        ss2 = self.small[:, 64:80]
        sd2 = self.small[:, 80:96]
        rstd2 = self.small[:, 96:112]
        ss3 = self.small[:, 112:128]
        sd3 = self.small[:, 128:144]
        rstd3 = self.small[:, 144:160]
        ps = self.ps
        RB = [0, 1, 2, 3]
        ACC = [4, 5, 6, 7]
        rr = [0]

        def rbank():
            b = RB[rr[0] % 4]
            rr[0] += 1
            return b
        self.table_cast(1000)
        for q in range(4):
            self.dma("pool", f"wq{q}", wq[:, q * 4:(q + 1) * 4, :],
                     self.wq_d[q * 512:(q + 1) * 512, :].rearrange("(kc p) n -> p kc n", p=128), [], [("wq", q)])
        self.dma("sp", "gbc", self.gbc[:], self.ln2_d[0:1, :].partition_broadcast(128), [], [("gbc",)])
        self.dma("sp", "c_gfc", gfc, self.lnf_d[0:1, :].partition_broadcast(128), [], [("gfc",)])
        self.dma("sp", "c_iota", iota, self.c_iota16_d.ap(), [], [("iota",)])
        self.dma("sp", "c_k1", scrA[:, 0:128], self.k1_d.ap(), [], [("scrA",)])
        self.dma("sp", "c_k2", scrA[:, 128:256], self.k2_d.ap(), [], [("scrA",)])
        self.cp(identf, self.ident[:], [("ident",)], [("identf",)], eng="dve")
        for j in range(2):
            b = rbank()
            self.mm(ps[b][:, 0:128], scrA[:, j * 128:(j + 1) * 128], identf, True, True, [("scrA",), ("identf",)], [("ps", b)])
            self.cp(kT[j], ps[b][:, 0:128], [("ps", b)], [("kT",)], eng="dve")

        def C(c):
            self.S.default_cost = c

        def prep(i):
            s2 = i % 2
            s3 = s2
            tl = slice(i * 128, (i + 1) * 128)
            self.dma("sp", f"h1b{s3}", h1b[s3], self.h1_d[tl, :], [("h1d", i)], [("h1b", s3)])
            C(0.75)
            self.stt(junk, h1b[s3], 1.0, h1b[s3], ALU.mult, ALU.mult, [("h1b", s3)], [("junk",), ("ss2", i)],
                     accum_out=ss2[:, i:i + 1])
            C(0.12)
            self.actf(sd2[:, i:i + 1], ss2[:, i:i + 1], AF.Ln, [("ss2", i), ("small",)], [("sd2", i)], bias=self.eps, scale=1.0 / D)
            self.actf(rstd2[:, i:i + 1], sd2[:, i:i + 1], AF.Exp, [("sd2", i)], [("rstd2", i)], scale=-0.5)
            C(0.75)
            self.stt(hnb[s2], h1b[s3], rstd2[:, i:i + 1], self.gbc[:], ALU.mult, ALU.mult,
                     [("h1b", s3), ("rstd2", i), ("gbc",)], [("hnb", s2)])
            C(0.2)
            for g4 in range(4):
                b = rbank()
                for j in range(4):
                    kc = g4 * 4 + j
                    self.mm(ps[b][:, j * 128:(j + 1) * 128], hnb[s2][:, kc * 128:(kc + 1) * 128], self.ident[:], True, True,
                            [("hnb", s2), ("ident",)], [("ps", b)])
                self.cp(hnT[:, g4 * 4:(g4 + 1) * 4, :], v3(ps[b][:, :], 128), [("ps", b)], [("hnT",)],
                        eng=("act" if g4 % 2 == 0 else "dve"))
            qT = v3(scrA, 128)
            for g4 in range(4):
                b = rbank()
                for j in range(4):
                    m = g4 * 4 + j
                    for kc in range(16):
                        self.mm(ps[b][:, j * 128:(j + 1) * 128], wq[:, kc, m * 128:(m + 1) * 128], hnT[:, kc, :],
                                kc == 0, kc == 15, [("wq", kc // 4), ("hnT",)], [("ps", b)])
                self.cp(qT[:, g4 * 4:(g4 + 1) * 4, :], v3(ps[b][:, :], 128), [("ps", b)], [("scrA",)],
                        eng=("act" if g4 % 2 == 0 else "dve"))
            sc = v3(scrB, 128)
            for g4 in range(4):
                b = rbank()
                for j in range(4):
                    m = g4 * 4 + j
                    self.mm(ps[b][:, j * 128:(j + 1) * 128], qT[:, m, :], kT[m % 2], True, True, [("scrA",), ("kT",)], [("ps", b)])
                self.cp(sc[:, g4 * 4:(g4 + 1) * 4, :], v3(ps[b][:, :], 128), [("ps", b)], [("scrB",)],
                        eng=("act" if g4 % 2 == 0 else "dve"))
            C(0.12)
            v16v = v3(v16, 16)
            i16v = v3(i16, 16)
            for m in range(16):
                self.top16(v16v[:, m, :], i16v[:, m, :], sc[:, m, :], tmpm[:, 0:128], ("scrB",), ("v16",), ("i16",), ("tmpm",))
            cand = self.bcap(scrA, [[256, 8], [16, 16], [1, 16]])
            v1b = self.bcap(v16, [[32, 8], [1, 16], [0, 16]])
            v2b = self.bcap(v16, [[32, 8], [0, 16], [1, 16]], off=16)
            C(0.75)
            self.tt(cand, v1b, v2b, ALU.add, [("v16",)], [("scrA",)])
            C(0.15)
            cand3 = v3(scrA, 256)
            tops3 = v3(tops, 16)
            pos3 = v3(pos, 16)
            for h in range(8):
                self.top16(tops3[:, h, :], pos3[:, h, :], cand3[:, h, :], tmpm, ("scrA",), ("tops",), ("pos",), ("tmpm",))
            self.cp(i16f, i16, [("i16",)], [("i16f",)], eng="dve")
            self.S.dve(lambda e: e.tensor_single_scalar(pab, pos, 4, ALU.logical_shift_right), [("pos",)], [("pab",)])
            self.cp(paf, pab, [("pab",)], [("paf",)], eng="dve")
            self.S.dve(lambda e: e.tensor_single_scalar(pab, pos, 15, ALU.bitwise_and), [("pos",), ("paf",)], [("pab",)])
            self.cp(pbf, pab, [("pab",)], [("pbf",)], eng="dve")
            eq4 = self.bcap(scrB, [[256, 8], [16, 16], [1, 16]])
            C(0.75)
            iob = self.bcap(iota, [[0, 8], [0, 16], [1, 16]])
            for (pf, off, dst, nm) in ((paf, 0, i1s, "i1s"), (pbf, 16, i2s, "i2s")):
                pfb = self.bcap(pf, [[16, 8], [1, 16], [0, 16]])
                ifb = self.bcap(i16f, [[32, 8], [0, 16], [1, 16]], off=off)
                self.tt(eq4, pfb, iob, ALU.is_equal, [("paf",), ("pbf",), ("iota",)], [("scrB",)])
                self.tt(eq4, eq4, ifb, ALU.mult, [("scrB",), ("i16f",)], [("scrB",)])
                self.S.dve(lambda e, dst=dst: e.tensor_reduce(dst, v3(scrB, 16), AX.X, ALU.add), [("scrB",)], [(nm,)])
            C(0.12)
            self.stt(ef, i1s, 128.0, i2s, ALU.mult, ALU.add, [("i1s",), ("i2s",)], [("ef",)])
            self.cp(eidx[s3], ef, [("ef",)], [("eidx", s3)], eng="dve")
            g3 = v3(gates[s3], 16)
            self.tt(g3, tops3, self.bcap(tops, [[16, 8], [0, 16]]), ALU.subtract, [("tops",)], [("gates", s3)])
            self.actf(gates[s3], gates[s3], AF.Exp, [("gates", s3)], [("gates", s3)])
            self.S.dve(lambda e: e.tensor_reduce(gs, g3, AX.X, ALU.add), [("gates", s3)], [("gs",)])
            self.recip(rg, gs, [("gs",)], [("rg",)])
            self.tt(g3, g3, self.bcap(rg, [[1, 8], [0, 16]]), ALU.mult, [("gates", s3), ("rg",)], [("gates", s3)])
            if i == 0:
                self.tap("eidx0", eidx[0], [128, 128], I32, [("eidx", 0)])
                self.tap("gates0", gates[0], [128, 128], F32, [("gates", 0)])

        def step(i, j):
            s2 = i % 2
            k = (i * 128 + j) % NB
            if j == 0:
                self.memset(actp[s2], 0.0, [("actp", s2, jj) for jj in range(128)])
            self.S.dma("pool", f"gb{k}", lambda e: e.indirect_dma_start(
                out=gb[k], out_offset=None, in_=self.uv16_d[:, :],
                in_offset=bass.IndirectOffsetOnAxis(ap=eidx[s2][:, j:j + 1], axis=0)),
                [("eidx", s2), ("tabu",), ("tabv",)], [("gbu", k), ("gbv", k)])
            if j % 4 == 0 or (j < 24 and j % 2 == 0):
                self.stt(gb[k][:, 0:D], gb[k][:, 0:D], 1.0, hnb[s2], ALU.mult, ALU.mult, [("gbu", k), ("hnb", s2)],
                         [("gbu", k), ("actp", s2, j)], accum_out=actp[s2][:, j:j + 1])
            else:
                self.tt(gb[k][:, 0:D], gb[k][:, 0:D], hnb[s2], ALU.mult, [("gbu", k), ("hnb", s2)], [("gbu", k)])
                self.actf(gb[k][:, 0:D], gb[k][:, 0:D], AF.Identity, [("gbu", k)], [("gbu", k), ("actp", s2, j)],
                          accum_out=actp[s2][:, j:j + 1])
            self.actf(gl[:, j:j + 1], actp[s2][:, j:j + 1], AF.Gelu, [("actp", s2, j)], [("gl", j)])

        def step2(i, j):
            s2 = i % 2
            k = (i * 128 + j) % NB
            d = dg[j % 4]
            self.ts(d, self.ident[:], gl[:, j:j + 1], gates[s2][:, j:j + 1], ALU.mult, ALU.mult,
                    [("ident",), ("gl", j), ("gates", s2)], [("dg", j % 4)])
            for n in range(4):
                self.mm(ps[ACC[n]][:, :], d, gb[k][:, D + n * 512:D + (n + 1) * 512], j == 0, j == 127,
                        [("dg", j % 4), ("gbv", k)], [("ps", ACC[n])])

        def finish_acc(i):
            s2 = i % 2
            for n in range(4):
                self.tt(h1b[s2][:, n * 512:(n + 1) * 512], ps[ACC[n]][:, :], h1b[s2][:, n * 512:(n + 1) * 512], ALU.add,
                        [("ps", ACC[n]), ("h1b", s2)], [("h1b", s2)])

        def finish_tile(i):
            s2 = i % 2
            tl = slice(i * 128, (i + 1) * 128)
            C(0.75)
            self.stt(junk, h1b[s2], 1.0, h1b[s2], ALU.mult, ALU.mult, [("h1b", s2)], [("junk",), ("ss3", i)],
                     accum_out=ss3[:, i:i + 1])
            self.actf(sd3[:, i:i + 1], ss3[:, i:i + 1], AF.Ln, [("ss3", i), ("small",)], [("sd3", i)], bias=self.eps, scale=1.0 / D)
            self.actf(rstd3[:, i:i + 1], sd3[:, i:i + 1], AF.Exp, [("sd3", i)], [("rstd3", i)], scale=-0.5)
            self.stt(h1b[s2], h1b[s2], rstd3[:, i:i + 1], gfc, ALU.mult, ALU.mult, [("h1b", s2), ("rstd3", i), ("gfc",)], [("h1b", s2)])
            self.dma("sp", f"out{s2}", self.out_d[tl, :], h1b[s2], [("h1b", s2)], [("outd", i)])
            C(0.12)

        ntile = self.peer_tiles
        prep(0)
        for st in range(ntile):
            if st >= 1:
                finish_acc(st - 1)
            pops = self.capture(lambda: finish_tile(st - 1)) if st >= 1 else []
            pops += self.capture(lambda: prep(st + 1)) if st + 1 < ntile else []
            plan = self.splice_plan(pops, 120)
            for j in range(128):
                step(st, j)
                if j >= 1:
                    step2(st, j - 1)
                self.S.ops.extend(plan.get(j, []))
            step2(st, 127)
        finish_acc(ntile - 1)
        finish_tile(ntile - 1)

    def build(self):
        self.declare()
        W = self.w_in_d
        self.wplan = []
        for half in range(2):
            self.wplan += [(W, half * 512, 512), (W, 1024 + half * 512, 512), (W, 2048 + half * 512, 512)]
        self.wplan += [(W, 5120, 16)]
        for hp in range(2):
            self.wplan += [(W, 3072 + hp * 256, 256), (W, 3584 + hp * 256, 256), (W, 4096 + hp * 512, 512), (W, 5136 + hp * 512, 512)]
        self._wnext = 0
        self._wtaken = 0
        self.phase0()
        self.phase1()
        self.m_persist = self.A.mark()
        self.tap("xnT0", self.xnT[:, 0, :], [128, S], BF16, [("xnT", i) for i in range(NT)])
        if self.stop_after == "p1":
            return self.finish()
        for half in range(2):
            self.attention_half(half)
        if self.stop_after != "attn":
            self.gla_setup()
            for hp in range(2):
                self.gla_pair(hp)
        if self.stop_after in ("attn", "gla"):
            t = self.nc.dram_tensor("tap_mixT", [D, S], BF16, kind="ExternalOutput")
            self.tap_names.append("tap_mixT")
            nh = 16 if self.stop_after == "attn" else 18
            self.dma("sp", "tap_mixT", t.ap(), self.mixT_d.ap(), [("mixT", h) for h in range(nh)], [("tapd", "mixT")])
            return self.finish()
        self.A.release(self.m_persist)
        self.S.barrier()
        self.phase5()
        if self.stop_after == "h1":
            t = self.nc.dram_tensor("tap_h1", [S, D], F32, kind="ExternalOutput")
            self.tap_names.append("tap_h1")
            self.dma("sp", "tap_h1", t.ap(), self.h1_d.ap(), [("h1d", i) for i in range(NT)], [("tapd", "h1")])
            return self.finish()
        self.A.release(0)
        self.S.barrier()
        self.peer()
        return self.finish()

    def finish(self):
        nc = self.nc
        Sd = self.S
        Sd.analyse()
        es = self.es
        sems = {e: es.enter_context(nc.semaphore("s_" + e)) for e in ENGS}
        dsems = {g: es.enter_context(nc.semaphore("d_" + g)) for g in Sd.dma_count}
        with nc.Block() as block:
            @block.sync
            def _(e):
                known = Sd.emit_engine("sp", e, sems, dsems)
                Sd.final_waits("sp", e, sems, dsems, known)

            @block.scalar
            def _(e):
                Sd.emit_engine("act", e, sems, dsems)

            @block.vector
            def _(e):
                Sd.emit_engine("dve", e, sems, dsems)

            @block.gpsimd
            def _(e):
                Sd.emit_engine("pool", e, sems, dsems)

            @block.tensor
            def _(e):
                Sd.emit_engine("pe", e, sems, dsems)
        self.es.close()
        return nc


_CONSTS = None


def make_in_map(inputs, b):
    global _CONSTS
    if _CONSTS is None:
        _CONSTS = _consts()
    f = lambda a: np.ascontiguousarray(np.asarray(a, dtype=np.float32))
    m = {
        "x": f(inputs["x"][b]),
        "ln1_g": f(inputs["ln1_g"]).reshape(1, D),
        "w_in": f(inputs["w_in"][0]),
        "rel_bias": f(inputs["rel_bias"]),
        "gla_w_gate2": f(inputs["gla_w_gate2"][0]),
        "gla_b_gate": f(inputs["gla_b_gate"]).reshape(1, 512),
        "gla_norm_g": f(inputs["gla_norm_g"][0]),
        "w_out": f(inputs["w_out"][0]),
        "ln2_g": f(inputs["ln2_g"]).reshape(1, D),
        "peer_w_query": f(inputs["peer_w_query"][0]),
        "peer_keys1": f(inputs["peer_keys1"][0]),
        "peer_keys2": f(inputs["peer_keys2"][0]),
        "peer_u": f(inputs["peer_u"][0]),
        "peer_v": f(inputs["peer_v"][0]),
        "ln_f_g": f(inputs["ln_f_g"]).reshape(1, D),
    }
    m.update(_CONSTS)
    return m


_NC_CACHE = {}


def kernel(**inputs):
    if "nc" not in _NC_CACHE:
        _NC_CACHE["nc"] = Builder().build()
    nc = _NC_CACHE["nc"]
    n = 8
    in_maps = [make_in_map(inputs, b) for b in range(n)]
    res = run_bass_kernel_spmd(nc, in_maps, core_ids=list(range(n)))
    out = np.stack([np.asarray(r["out"], dtype=np.float32) for r in res.results], axis=0)
    return out
```

```python
import math
from contextlib import ExitStack

import numpy as np
import ml_dtypes

import concourse.bass as bass
import concourse.mybir as mybir
from concourse.bass_utils import run_bass_kernel_spmd

F32 = mybir.dt.float32
BF16 = mybir.dt.bfloat16
I32 = mybir.dt.int32
U32 = mybir.dt.uint32
AF = mybir.ActivationFunctionType
ALU = mybir.AluOpType
AX = mybir.AxisListType

D = 2048
S = 2048
NT = 16
D_IN = 6160
EPS = 1e-6
LTAB = 2560
NEXP = 16384

ENGS = ("pe", "act", "dve", "pool", "sp")


class Sched:
    def __init__(self):
        self.ops = []

    default_cost = 0.12

    def add(self, eng, fn, reads=(), writes=(), dma=None, cost=None):
        self.ops.append(dict(kind="op", eng=eng, fn=fn, reads=tuple(reads), writes=tuple(writes), dma=dma,
                             cost=self.default_cost if cost is None else cost))

    def pe(self, fn, reads=(), writes=()):
        self.add("pe", fn, reads, writes)

    def act(self, fn, reads=(), writes=()):
        self.add("act", fn, reads, writes)

    def dve(self, fn, reads=(), writes=()):
        self.add("dve", fn, reads, writes)

    def pool(self, fn, reads=(), writes=()):
        self.add("pool", fn, reads, writes)

    def dma(self, queue, group, fn, reads=(), writes=()):
        self.add(queue, fn, reads, writes, dma=group)

    def barrier(self):
        self.ops.append(dict(kind="barrier"))

    def analyse(self):
        ops = self.ops
        last_writer = {}
        readers = {}
        eng_pos = {e: 0 for e in ENGS}
        eng_last = {e: None for e in ENGS}
        dma_count = {}
        pending_bar = {e: None for e in ENGS}
        for i, op in enumerate(ops):
            if op["kind"] == "barrier":
                info = dict(eng={e: eng_last[e] for e in ENGS if eng_last[e] is not None}, dma=dict(dma_count))
                for oid in info["eng"].values():
                    ops[oid]["signal"] = True
                for e in ENGS:
                    if pending_bar[e] is None:
                        pending_bar[e] = info
                    else:
                        pending_bar[e] = info
                last_writer.clear()
                readers.clear()
                continue
            e = op["eng"]
            op["pos"] = eng_pos[e]
            eng_pos[e] += 1
            op["signal"] = op.get("signal", False)
            deps = set()
            for r in op["reads"]:
                w = last_writer.get(r)
                if w is not None:
                    deps.add((w, "raw"))
            for wr in op["writes"]:
                w = last_writer.get(wr)
                if w is not None:
                    deps.add((w, "waw"))
                for rd in readers.get(wr, ()):
                    deps.add((rd, "war"))
            waits = []
            best_eng = {}
            best_dma = {}
            for (pid, kind) in deps:
                if pid == i:
                    continue
                p = ops[pid]
                if p["dma"] is not None:
                    if p["dma_idx"] > best_dma.get(p["dma"], 0):
                        best_dma[p["dma"]] = p["dma_idx"]
                    continue
                if p["eng"] == e and op["dma"] is None:
                    if e == "pe":
                        continue
                if pid > best_eng.get(p["eng"], -1):
                    best_eng[p["eng"]] = pid
            for e2, pid in best_eng.items():
                ops[pid]["signal"] = True
                waits.append(("eng", e2, pid))
            for g, c in best_dma.items():
                waits.append(("dma", g, c))
            if pending_bar[e] is not None:
                info = pending_bar[e]
                for e2, oid in info["eng"].items():
                    if not (e2 == e == "pe"):
                        waits.append(("eng", e2, oid))
                for g, c in info["dma"].items():
                    waits.append(("dma", g, c))
                pending_bar[e] = None
            op["waits"] = waits
            if op["dma"] is not None:
                g = op["dma"]
                dma_count[g] = dma_count.get(g, 0) + 1
                op["dma_idx"] = dma_count[g]
            else:
                eng_last[e] = i
            for r in op["reads"]:
                readers.setdefault(r, []).append(i)
            for wr in op["writes"]:
                last_writer[wr] = i
                readers[wr] = []
        self.dma_count = dma_count
        cnt = {e: 0 for e in ENGS}
        for op in ops:
            if op["kind"] != "op" or op["dma"] is not None:
                continue
            if op["signal"]:
                cnt[op["eng"]] += 1
                op["sigval"] = cnt[op["eng"]]
        self.sig_count = cnt

    def emit_engine(self, ename, eng, sems, dsems):
        known = {}
        n = 0
        for op in self.ops:
            if op["kind"] != "op" or op["eng"] != ename:
                continue
            need = {}
            for w in op["waits"]:
                if w[0] == "dma":
                    key = ("dma", w[1])
                    val = 16 * w[2]
                else:
                    key = ("eng", w[1])
                    val = self.ops[w[2]]["sigval"]
                if val > need.get(key, 0):
                    need[key] = val
            for key, val in need.items():
                if known.get(key, 0) >= val:
                    continue
                known[key] = val
                sem = dsems[key[1]] if key[0] == "dma" else sems[key[1]]
                eng.wait_ge(sem, val)
            ins = op["fn"](eng)
            if op["dma"] is not None:
                ins.then_inc(dsems[op["dma"]], 16)
            elif op["signal"]:
                ins.then_inc(sems[ename], 1)
            n += 1
        return known

    def final_waits(self, ename, eng, sems, dsems, known):
        for g, c in self.dma_count.items():
            if known.get(("dma", g), 0) < 16 * c:
                eng.wait_ge(dsems[g], 16 * c)
        for e2, c in self.sig_count.items():
            if e2 != ename and c > 0 and known.get(("eng", e2), 0) < c:
                eng.wait_ge(sems[e2], c)


def _t5_bucket(dist):
    dist = np.asarray(dist, dtype=np.int64)
    nf = np.maximum(dist, 1).astype(np.float32)
    large = 16 + (np.log(nf / np.float32(16)) / np.float32(math.log(2048 / 16)) * np.float32(16)).astype(np.int32)
    large = np.minimum(large, 31)
    return np.where(dist < 16, dist, large)


def _consts():
    c = {}
    c["c_ident"] = np.eye(128, dtype=np.float32).astype(ml_dtypes.bfloat16)
    y = np.arange(LTAB)
    dist = y - 511
    mult = ((dist >= 0) & (dist <= 128)).astype(np.int64) \
        + ((dist >= 0) & (dist <= 512) & (dist % 4 == 0)).astype(np.int64) \
        + ((dist >= 0) & (dist <= 2048) & (dist % 16 == 0)).astype(np.int64)
    bucket = _t5_bucket(np.maximum(dist, 0))
    oh = np.zeros((32, LTAB), dtype=np.float32)
    valid = mult > 0
    oh[bucket[valid], y[valid]] = mult[valid].astype(np.float32)
    c["c_onehot"] = oh.astype(ml_dtypes.bfloat16)
    t = np.arange(128)
    incl = (t[:, None] <= t[None, :]).astype(np.float32)
    ref = (t[:, None] <= 63).astype(np.float32)
    s = np.float32(-1.0 / 16.0)
    g = np.zeros((128, 640), dtype=np.float32)
    g[:, 0:128] = s * (incl - ref)
    g[:, 128:256] = s * incl
    g[:, 256] = s
    g[:, 384:512] = s * (t[:, None] > t[None, :]).astype(np.float32)
    g[:, 512:640] = (t[:, None] <= t[None, :]).astype(np.float32)
    c["c_gla"] = g
    c["c_iota16"] = np.tile(np.arange(16, dtype=np.float32)[None, :], (128, 1))
    return c


class Arena:
    def __init__(self, t, nwords):
        self.t = t
        self.n = nwords
        self.off = 0
        self.peak = 0

    def f32(self, n):
        assert self.off + n <= self.n, f"arena overflow {self.off}+{n}>{self.n}"
        ap = self.t[:, self.off:self.off + n]
        self.off += n
        self.peak = max(self.peak, self.off)
        return ap

    def bf16(self, n):
        w = (n + 1) // 2
        return self.f32(w).bitcast(BF16)[:, 0:n]

    def i32(self, n):
        return self.f32(n).bitcast(I32)

    def u32(self, n):
        return self.f32(n).bitcast(U32)

    def mark(self):
        return self.off

    def release(self, m):
        self.off = m


def v3(ap, b):
    return ap.rearrange("p (a b) -> p a b", b=b)


class Builder:
    def __init__(self, taps=(), stop_after=None, peer_tiles=NT):
        self.peer_tiles = peer_tiles
        self.taps = set(taps)
        self.stop_after = stop_after
        self.S = Sched()
        self.nc = bass.Bass("TRN2", target_bir_lowering=False)
        self.es = ExitStack()
        self.tap_names = []
        self._bank_rr = 0

    def mm(self, out, lhsT, rhs, start, stop, reads, writes):
        self.S.pe(lambda e: e.matmul(out, lhsT, rhs, start=start, stop=stop), reads, writes)

    def actf(self, out, in_, func, reads, writes, bias=None, scale=None, accum_out=None, eng="act"):
        kw = {}
        if bias is not None:
            kw["bias"] = bias
        if scale is not None:
            kw["scale"] = scale
        if accum_out is not None:
            kw["accum_out"] = accum_out
        self.S.act(lambda e: e.activation(out, in_, func, **kw), reads, writes)

    def tt(self, out, in0, in1, op, reads, writes, eng="dve"):
        self.S.add(eng, lambda e: e.tensor_tensor(out, in0, in1, op), reads, writes)

    def ts(self, out, in0, s1, s2, op0, op1, reads, writes, eng="dve", accum_out=None):
        if op1 is None:
            self.S.add(eng, lambda e: e.tensor_scalar(out, in0, s1, None, op0), reads, writes)
        elif accum_out is None:
            self.S.add(eng, lambda e: e.tensor_scalar(out, in0, s1, s2, op0, op1), reads, writes)
        else:
            self.S.add(eng, lambda e: e.tensor_scalar(out, in0, s1, s2, op0, op1, accum_out), reads, writes)

    def stt(self, out, in0, scalar, in1, op0, op1, reads, writes, eng="dve", accum_out=None):
        if accum_out is None:
            self.S.add(eng, lambda e: e.scalar_tensor_tensor(out, in0, scalar, in1, op0, op1), reads, writes)
        else:
            self.S.add(eng, lambda e: e.scalar_tensor_tensor(out, in0, scalar, in1, op0, op1, accum_out), reads, writes)

    def cp(self, out, in_, reads, writes, eng="dve"):
        if eng == "act":
            self.S.act(lambda e: e.copy(out, in_), reads, writes)
        else:
            self.S.add(eng, lambda e: e.tensor_copy(out, in_), reads, writes)

    def recip(self, out, in_, reads, writes):
        self.S.dve(lambda e: e.reciprocal(out, in_), reads, writes)

    def memset(self, ap, val, writes, eng="dve"):
        self.S.add(eng, lambda e: e.memset(ap, val), (), writes)

    def dma(self, queue, group, out, in_, reads, writes, **kw):
        self.S.dma(queue, group, lambda e: e.dma_start(out=out, in_=in_, **kw), reads, writes)

    def tap(self, name, src, shape, dtype, reads):
        if name not in self.taps:
            return
        t = self.nc.dram_tensor("tap_" + name, list(shape), dtype, kind="ExternalOutput")
        self.tap_names.append("tap_" + name)
        self.dma("sp", "tap_" + name, t.ap(), src, reads, [("tapd", name)])

    def bank(self):
        b = self._bank_rr % 8
        self._bank_rr += 1
        return b

    def declare(self):
        nc = self.nc
        di = lambda n, s, d=F32: nc.dram_tensor(n, list(s), d, kind="ExternalInput")
        self.x_d = di("x", [S, D])
        self.ln1_d = di("ln1_g", [1, D])
        self.w_in_d = di("w_in", [D, D_IN])
        self.rb_d = di("rel_bias", [32, 16])
        self.wg2_d = di("gla_w_gate2", [16, 512])
        self.bg_d = di("gla_b_gate", [1, 512])
        self.gn_d = di("gla_norm_g", [4, 256])
        self.w_out_d = di("w_out", [D, D])
        self.ln2_d = di("ln2_g", [1, D])
        self.wq_d = di("peer_w_query", [D, D])
        self.k1_d = di("peer_keys1", [128, 128])
        self.k2_d = di("peer_keys2", [128, 128])
        self.u_d = di("peer_u", [NEXP, D])
        self.v_d = di("peer_v", [NEXP, D])
        self.lnf_d = di("ln_f_g", [1, D])
        self.c_ident_d = di("c_ident", [128, 128], BF16)
        self.c_onehot_d = di("c_onehot", [32, LTAB], BF16)
        self.c_gla_d = di("c_gla", [128, 640])
        self.c_iota16_d = di("c_iota16", [128, 16])
        self.out_d = nc.dram_tensor("out", [S, D], F32, kind="ExternalOutput")
        self.ebscr_d = nc.dram_tensor("ebscr", [16, 128, LTAB + 1], BF16, kind="Internal")
        self.mixT_d = nc.dram_tensor("mixT", [D, S], BF16, kind="Internal")
        self.h1_d = nc.dram_tensor("h1", [S, D], F32, kind="Internal")
        self.uv16_d = nc.dram_tensor("uv16", [NEXP, 2 * D], BF16, kind="Internal")
        self._tc_pending = []
        for r0 in range(0, NEXP, 512):
            self._tc_pending.append((self.u_d, 0, "tcu", "tabu", r0))
            self._tc_pending.append((self.v_d, D, "tcv", "tabv", r0))

        es = self.es
        sb = lambda n, s, d: es.enter_context(nc.sbuf_tensor(n, list(s), d))
        self.ident = sb("ident", [128, 128], BF16)
        self.gbc = sb("gbc", [128, D], F32)
        self.small = sb("small", [128, 256], F32)
        self.ones_f = sb("ones_f", [128, 128], F32)
        self.ones_b = sb("ones_b", [128, 128], BF16)
        ARENA_WORDS = 49 * 1024
        self.arena_t = sb("arena", [128, ARENA_WORDS], F32)
        self.A = Arena(self.arena_t, ARENA_WORDS)
        self.ps = [es.enter_context(nc.psum_tensor(f"ps{b}", [128, 512], F32)) for b in range(8)]

    def phase0(self):
        A = self.A
        m0 = A.mark()
        self.dma("sp", "c_ident", self.ident[:], self.c_ident_d.ap(), [], [("ident",)])
        self.dma("sp", "gbc", self.gbc[:], self.ln1_d[0:1, :].partition_broadcast(128), [], [("gbc",)])
        self.memset(self.ones_f[:], 1.0, [("ones_f",)])
        self.memset(self.ones_b[:], 1.0, [("ones_b",)])
        self.memset(self.small[:], 0.0, [("small",)])
        self.eps = self.small[:, 255:256]
        self.memset(self.eps, EPS, [("small",)])
        rb = A.f32(16)
        erb = A.bf16(16)
        oh = A.bf16(LTAB)
        rbh = A.bf16(128)
        reps = [A.bf16(LTAB), A.bf16(LTAB)]
        self.dma("sp", "c_rb", rb[0:32, :], self.rb_d.ap(), [], [("rb",)])
        self.dma("sp", "c_oh", oh[0:32, :], self.c_onehot_d.ap(), [], [("oh",)])
        self.actf(erb[0:32, :], rb[0:32, :], AF.Exp, [("rb",)], [("erb",)])
        for h in range(16):
            rep = reps[h % 2]
            self.cp(rbh[0:32, :], erb[0:32, h:h + 1].to_broadcast([32, 128]), [("erb",)], [("rbh",)])
            for ch in range(LTAB // 512):
                b = self.bank()
                sl = slice(ch * 512, (ch + 1) * 512)
                self.mm(self.ps[b][:, :], rbh[0:32, :], oh[0:32, sl], True, True, [("rbh",), ("oh",)], [("ps", b)])
                self.cp(rep[:, sl], self.ps[b][:, :], [("ps", b)], [("rep", h % 2)], eng=("act" if ch % 2 == 0 else "dve"))
            self.dma("sp", f"rep{h % 2}", self.ebscr_d[h, :, 0:LTAB], rep, [("rep", h % 2)], [("ebscr", h)])
        self.S.barrier()
        A.release(m0)

    def phase1(self):
        A = self.A
        self.xnT = v3(A.bf16(16 * S), S)
        self.wbuf = [v3(A.bf16(16 * 512), 512), v3(A.bf16(16 * 512), 512)]
        self.wissue()
        self.wissue()
        m0 = A.mark()
        NX = 4
        xin = [A.f32(D) for _ in range(NX)]
        xnb = [A.bf16(D), A.bf16(D)]
        junk = A.bf16(D)
        ss = self.small[:, 0:16]
        sd = self.small[:, 16:32]
        rstd = self.small[:, 32:48]

        def load(i):
            s = i % NX
            self.dma("sp", f"xin{s}", xin[s], self.x_d[i * 128:(i + 1) * 128, :], [], [("xin", s)])

        def stats(i):
            s = i % NX
            self.actf(junk, xin[s], AF.Square, [("xin", s)], [("junk",), ("ss", i)], accum_out=ss[:, i:i + 1])
            self.actf(sd[:, i:i + 1], ss[:, i:i + 1], AF.Ln, [("ss", i), ("small",)], [("sd", i)],
                      bias=self.eps, scale=1.0 / D)
            self.actf(rstd[:, i:i + 1], sd[:, i:i + 1], AF.Exp, [("sd", i)], [("rstd", i)], scale=-0.5)

        def norm_t(i):
            s = i % NX
            xb = xnb[i % 2]
            self.stt(xb, xin[s], rstd[:, i:i + 1], self.gbc[:], ALU.mult, ALU.mult,
                     [("xin", s), ("rstd", i), ("gbc",)], [("xnb", i % 2)])
            for g4 in range(4):
                b = self.bank()
                for j in range(4):
                    kc = g4 * 4 + j
                    self.mm(self.ps[b][:, j * 128:(j + 1) * 128], xb[:, kc * 128:(kc + 1) * 128], self.ident[:],
                            True, True, [("xnb", i % 2), ("ident",)], [("ps", b)])
                self.cp(self.xnT[:, g4 * 4:(g4 + 1) * 4, i * 128:(i + 1) * 128], v3(self.ps[b][:, :], 128),
                        [("ps", b)], [("xnT", i)], eng=("act" if g4 % 2 == 0 else "dve"))

        for i in range(min(3, NT)):
            load(i)
        stats(0)
        for i in range(NT):
            if i + 3 < NT:
                load(i + 3)
            if i + 1 < NT:
                stats(i + 1)
            norm_t(i)
        self.tap("small", self.small[:, 0:48], [128, 48], F32, [("rstd", i) for i in range(NT)])
        A.release(m0)
        self.S.barrier()

    def wissue(self):
        if self._wnext >= len(self.wplan):
            return
        w_d, col0, ncols = self.wplan[self._wnext]
        self.wload(w_d, col0, ncols, self._wnext % 2)
        self._wnext += 1

    def wtake(self, col0, ncols):
        k = self._wtaken
        assert self.wplan[k][1] == col0 and self.wplan[k][2] == ncols, (self.wplan[k], col0, ncols)
        self._wtaken += 1
        return k % 2

    def wload(self, w_d, col0, ncols, slot):
        wb = self.wbuf[slot]
        for q in range(4):
            src = w_d[q * 512:(q + 1) * 512, col0:col0 + ncols].rearrange("(kc p) n -> p kc n", p=128)
            self.dma("pool", f"w{slot}", wb[:, q * 4:(q + 1) * 4, 0:ncols], src, [], [("wbuf", slot)])

    def proj_fm(self, slot, ncols, evac):
        wb = self.wbuf[slot]
        for m in range((ncols + 127) // 128):
            mw = min(128, ncols - m * 128)
            for tc in range(4):
                b = self.bank()
                for kc in range(16):
                    self.mm(self.ps[b][0:mw, :], wb[:, kc, m * 128:m * 128 + mw], self.xnT[:, kc, tc * 512:(tc + 1) * 512],
                            kc == 0, kc == 15, [("wbuf", slot)] + [("xnT", 4 * tc + j) for j in range(4)], [("ps", b)])
                evac(m, tc, self.ps[b], b)

    def proj_tm(self, slot, ncols, evac):
        wb = self.wbuf[slot]
        for i in range(NT):
            b = self.bank()
            for kc in range(16):
                self.mm(self.ps[b][:, 0:ncols], self.xnT[:, kc, i * 128:(i + 1) * 128], wb[:, kc, 0:ncols],
                        kc == 0, kc == 15, [("wbuf", slot), ("xnT", i)], [("ps", b)])
            evac(i, self.ps[b], b)

    def attention_half(self, half):
        A = self.A
        m0 = A.mark()
        aqT = v3(A.bf16(4 * S), S)
        akT = v3(A.bf16(4 * S), S)
        av = A.bf16(16 * 8 * 65).rearrange("p (i h e) -> p i h e", h=8, e=65)
        EB = [A.bf16(2432) for _ in range(4)]
        E = [A.bf16(512) for _ in range(4)]
        P = [A.bf16(512) for _ in range(4)]
        ostage = [A.bf16(S), A.bf16(S)]
        rc = A.f32(512)
        bcs = A.f32(512)
        hs = f"h{half}"
        for nm, col0, dst in (("aq", half * 512, aqT), ("ak", 1024 + half * 512, akT)):
            slot = self.wtake(col0, 512)

            def evac(m, tc, ps, b, dst=dst, nm=nm):
                self.cp(dst[:, m, tc * 512:(tc + 1) * 512], ps[:, :], [("ps", b)], [(nm, m)],
                        eng=("act" if (m + tc) % 2 == 0 else "dve"))
            self.proj_fm(slot, 512, evac)
            self.wissue()
        slot = self.wtake(2048 + half * 512, 512)
        self.memset(av[:, :, :, 64:65], 1.0, [("av", i) for i in range(NT)])

        def evac_v(i, ps, b):
            self.cp(av[:, i, :, 0:64], v3(ps[:, :], 64), [("ps", b)], [("av", i)], eng=("act" if i % 2 == 0 else "dve"))
        self.proj_tm(slot, 512, evac_v)
        self.wissue()
        if half == 0:
            self.tap("aqT0", aqT[:, 0, :], [128, S], BF16, [("aq", 0)])
            self.tap("akT0", akT[:, 0, :], [128, S], BF16, [("ak", 0)])
            self.tap("av0", av[:, 0, :, :], [128, 8, 65], BF16, [("av", 0)])

        steps = []
        for pr in range(4):
            for c in range(4):
                for kb in range(4 * c + 4):
                    steps.append((pr, c, kb))
        SB = [0, 1, 2, 3]
        OBP = [4, 5, 7]
        BCB = 6
        ob_of = {}

        def load_eb(pr):
            for h2 in range(2):
                hh = pr * 2 + h2
                h = half * 8 + hh
                src = bass.AP(self.ebscr_d, h * 128 * (LTAB + 1) + 127, [[LTAB, 128], [1, 2432]])
                self.dma("sp", f"EB{hh % 4}", EB[hh % 4], src, [("ebscr", h)], [("EB", hh % 4)])

        def emit_S(n):
            pr, c, kb = steps[n]
            for h2 in range(2):
                hh = pr * 2 + h2
                base = h2 * 64
                b = SB[(2 * n + h2) % 4]
                if c == 0 and kb == 0 and h2 == 0 and pr + 1 < 4:
                    load_eb(pr + 1)
                self.mm(self.ps[b][:, :], akT[base:base + 64, pr, kb * 128:(kb + 1) * 128],
                        aqT[base:base + 64, pr, c * 512:(c + 1) * 512], True, True,
                        [("ak", pr), ("aq", pr)], [("ps", b)])

        def emit_rest(n):
            pr, c, kb = steps[n]
            nkb = 4 * c + 4
            for h2 in range(2):
                hh = pr * 2 + h2
                b = SB[(2 * n + h2) % 4]
                es = (2 * n + h2) % 4
                if kb == 0:
                    ob_of[(hh, c)] = OBP[len(ob_of) % 3]
                ob = ob_of[(hh, c)]
                self.actf(E[es], self.ps[b][:, :], AF.Exp, [("ps", b)], [("E", es)], scale=0.125)
                s0 = 512 * c - 128 * kb + 384
                self.tt(P[es], E[es], EB[hh % 4][:, s0:s0 + 512], ALU.mult, [("E", es), ("EB", hh % 4)], [("P", es)])
                self.mm(self.ps[ob][0:65, :], av[:, kb, hh, :], P[es], kb == 0, kb == nkb - 1,
                        [("av", kb), ("P", es)], [("ps", ob)])
            if kb == nkb - 1:
                for h2 in range(2):
                    hh = pr * 2 + h2
                    ob = ob_of[(hh, c)]
                    osl = hh % 2
                    self.actf(rc[64:65, :], self.ps[ob][64:65, :], AF.Ln, [("ps", ob)], [("rc",)])
                    self.actf(rc[64:65, :], rc[64:65, :], AF.Exp, [("rc",)], [("rc",)], scale=-1.0)
                    self.mm(self.ps[BCB][0:64, :], self.ones_f[64:65, 0:64], rc[64:65, :], True, True,
                            [("rc",), ("ones_f",)], [("ps", BCB)])
                    self.cp(bcs[0:64, :], self.ps[BCB][0:64, :], [("ps", BCB)], [("bcs",)], eng="act")
                    self.tt(ostage[osl][0:64, c * 512:(c + 1) * 512], self.ps[ob][0:64, :], bcs[0:64, :], ALU.mult,
                            [("ps", ob), ("bcs",)], [("ostage", osl)])
                    if c == 3:
                        h = half * 8 + hh
                        self.dma("sp", f"ost{osl}", self.mixT_d[h * 64:(h + 1) * 64, :], ostage[osl][0:64, :],
                                 [("ostage", osl)], [("mixT", h)])

        load_eb(0)
        self.table_cast(26)
        LOOK = 1
        for n in range(min(LOOK, len(steps))):
            emit_S(n)
        for n in range(len(steps)):
            if n + LOOK < len(steps):
                emit_S(n + LOOK)
            emit_rest(n)
        A.release(m0)
        self.S.barrier()


    def gla_setup(self):
        A = self.A
        self.gaT = A.f32(S)
        self.w2 = A.f32(512)
        self.bg = A.f32(512)
        self.gn = A.f32(8)
        self.cg = A.f32(640)
        self.dma("sp", "c_w2", self.w2[0:16, :], self.wg2_d.ap(), [], [("w2",)])
        self.dma("sp", "c_bg", self.bg[0:1, :], self.bg_d.ap(), [], [("bg",)])
        self.dma("sp", "c_gn", self.gn, self.gn_d.ap().rearrange("h (c p) -> p h c", p=128), [], [("gn",)],
                 allow_slow_non_contiguous=True)
        self.dma("sp", "c_cg", self.cg, self.c_gla_d.ap(), [], [("cg",)])
        slot = self.wtake(5120, 16)

        def evac(m, tc, ps, b):
            self.cp(self.gaT[0:16, tc * 512:(tc + 1) * 512], ps[0:16, :], [("ps", b)], [("gaT",)], eng="dve")
        self.proj_fm(slot, 16, evac)
        self.wissue()

    def gla_pair(self, hp):
        A = self.A
        m0 = A.mark()
        gqT = v3(A.bf16(2 * S), S)
        gkT = v3(A.bf16(2 * S), S)
        QtT = v3(A.bf16(2 * S), S)
        gkt = v3(A.bf16(16 * 256), 256)
        gvt = v3(A.bf16(16 * 512), 512)
        grT = v3(A.bf16(4 * S), S)
        ostage = grT
        dec = v3(A.f32(32), 2)
        la = [A.f32(256), A.f32(256)]
        ez = A.f32(256)
        e1 = A.f32(256)
        e1i = A.f32(256)
        e4 = A.f32(256)
        e3 = A.f32(258)
        Sf = v3(A.f32(512), 256)
        Sb = v3(A.bf16(512), 256)
        am = [A.bf16(128), A.bf16(128)]
        sq = A.f32(256)
        sdn = A.f32(128)
        rs = A.f32(128)
        t1 = A.f32(128)
        cg = self.cg
        W = self.w_in_d
        slot = self.wtake(3072 + hp * 256, 256)

        def ev_q(m, tc, ps, b):
            self.actf(gqT[:, m, tc * 512:(tc + 1) * 512], ps[:, :], AF.Copy, [("ps", b)], [("gqT", m)], scale=128.0 ** -0.5)
        self.proj_fm(slot, 256, ev_q)
        self.wissue()
        slot = self.wtake(3584 + hp * 256, 256)

        def ev_k(m, tc, ps, b):
            self.cp(gkT[:, m, tc * 512:(tc + 1) * 512], ps[:, :], [("ps", b)], [("gkT", m)],
                    eng=("act" if tc % 2 == 0 else "dve"))
        self.proj_fm(slot, 256, ev_k)

        def ev_kt(i, ps, b):
            self.cp(gkt[:, i, :], ps[:, 0:256], [("ps", b)], [("gkt", i)], eng=("act" if i % 2 == 0 else "dve"))
        self.proj_tm(slot, 256, ev_kt)
        self.wissue()
        slot = self.wtake(4096 + hp * 512, 512)

        def ev_v(i, ps, b):
            self.cp(gvt[:, i, :], ps[:, :], [("ps", b)], [("gvt", i)], eng=("act" if i % 2 == 0 else "dve"))
        self.proj_tm(slot, 512, ev_v)
        self.wissue()
        slot = self.wtake(5136 + hp * 512, 512)

        def ev_r(m, tc, ps, b):
            self.actf(grT[:, m, tc * 512:(tc + 1) * 512], ps[:, :], AF.Silu, [("ps", b)], [("grT", m)])
        self.proj_fm(slot, 512, ev_r)
        self.wissue()

        ZB, BA, BB, PA, PO, PK, PN = 0, 1, 2, 3, (4, 5), 6, 7
        ps = self.ps
        for i in range(NT):
            tl = slice(i * 128, (i + 1) * 128)
            l = la[i % 2]
            lr = ("la", i % 2)
            self.mm(ps[ZB][:, 0:256], self.gaT[0:16, tl], self.w2[0:16, hp * 256:(hp + 1) * 256], True, False,
                    [("gaT",), ("w2",)], [("ps", ZB)])
            self.mm(ps[ZB][:, 0:256], self.ones_f[0:1, 0:128], self.bg[0:1, hp * 256:(hp + 1) * 256], False, True,
                    [("ones_f",), ("bg",)], [("ps", ZB)])
            self.actf(ez, ps[ZB][:, 0:256], AF.Exp, [("ps", ZB)], [("ez",)], scale=-1.0)
            self.actf(l, ez, AF.Ln, [("ez",), ("ones_f",)], [lr], bias=self.ones_f[:, 0:1])
            for h2 in range(2):
                self.mm(ps[BA][:, h2 * 128:(h2 + 1) * 128], l[:, h2 * 128:(h2 + 1) * 128], cg[:, 0:128], True, True,
                        [lr, ("cg",)], [("ps", BA)])
                self.mm(ps[BB][:, h2 * 129:(h2 + 1) * 129], l[:, h2 * 128:(h2 + 1) * 128], cg[:, 128:257], True, True,
                        [lr, ("cg",)], [("ps", BB)])
            self.mm(ps[BA][:, 256:512], cg[:, 384:512], l, True, True, [lr, ("cg",)], [("ps", BA)])
            self.actf(e1, ps[BA][:, 0:256], AF.Exp, [("ps", BA)], [("e1",)])
            self.actf(e1i, ps[BA][:, 0:256], AF.Exp, [("ps", BA)], [("e1i",)], scale=-1.0)
            self.actf(e4, ps[BA][:, 256:512], AF.Exp, [("ps", BA)], [("e4",)])
            self.actf(e3, ps[BB][:, 0:258], AF.Exp, [("ps", BB)], [("e3",)])
            self.tt(QtT[:, :, tl], gqT[:, :, tl], v3(e1, 128), ALU.mult, [("gqT", 0), ("gqT", 1), ("e1",)], [("QtT", i)])
            self.tt(gkT[:, :, tl], gkT[:, :, tl], v3(e1i, 128), ALU.mult, [("gkT", 0), ("gkT", 1), ("e1i",)], [("KtT", i)])
            self.tt(gqT[:, :, tl], gqT[:, :, tl], v3(e3, 129)[:, :, 0:128], ALU.mult, [("gqT", 0), ("gqT", 1), ("e3",), ("QtT", i)],
                    [("QhT", i)])
            self.tt(gkt[:, i, :], gkt[:, i, :], e4, ALU.mult, [("gkt", i), ("e4",)], [("Kh", i)])
            self.cp(dec[:, i, :], v3(e3, 129)[:, :, 128], [("e3",)], [("dec", i)], eng="dve")
        self.table_cast(6)
        PA2 = (0, 1)
        PK2 = (2, 3)
        PN = 3
        POB = ((4, 5), (6, 7))

        def rec_A(i):
            tl = slice(i * 128, (i + 1) * 128)
            for h2 in range(2):
                self.mm(ps[PA2[h2]][:, 0:128], gkT[:, h2, tl], QtT[:, h2, tl], True, True, [("KtT", i), ("QtT", i)],
                        [("ps", PA2[h2])])
            for h2 in range(2):
                self.tt(am[h2], ps[PA2[h2]][:, 0:128], cg[:, 512:640], ALU.mult, [("ps", PA2[h2]), ("cg",)], [("am", h2)])
            for h2 in range(2):
                po = POB[h2][i % 2]
                if i < NT - 1:
                    self.mm(ps[PK2[h2]][:, 0:256], gkt[:, i, h2 * 128:(h2 + 1) * 128], gvt[:, i, h2 * 256:(h2 + 1) * 256],
                            True, True, [("Kh", i), ("gvt", i)], [("ps", PK2[h2])])
                for vc in range(2):
                    osl = ps[po][:, vc * 128:(vc + 1) * 128]
                    if i > 0:
                        self.mm(osl, Sb[:, h2, vc * 128:(vc + 1) * 128], gqT[:, h2, tl], True, False,
                                [("Sb", h2), ("QhT", i)], [("ps", po)])
                    self.mm(osl, gvt[:, i, h2 * 256 + vc * 128:h2 * 256 + (vc + 1) * 128], am[h2], i == 0, True,
                            [("gvt", i), ("am", h2)], [("ps", po)])
            if i < NT - 1:
                for h2 in range(2):
                    if i == 0:
                        self.cp(Sf[:, h2, :], ps[PK2[h2]][:, 0:256], [("ps", PK2[h2])], [("Sf", h2)], eng="dve")
                    else:
                        self.stt(Sf[:, h2, :], Sf[:, h2, :], dec[:, i, h2:h2 + 1], ps[PK2[h2]][:, 0:256], ALU.mult, ALU.add,
                                 [("Sf", h2), ("dec", i), ("ps", PK2[h2])], [("Sf", h2)])
                    self.cp(Sb[:, h2, :], Sf[:, h2, :], [("Sf", h2)], [("Sb", h2)], eng="act")

        def rec_N(i):
            tl = slice(i * 128, (i + 1) * 128)
            for h2 in range(2):
                po = POB[h2][i % 2]
                self.actf(sq, ps[po][:, 0:256], AF.Square, [("ps", po)], [("sq",)])
                self.mm(ps[PN][:, 256:384], self.ones_f[:, 0:128], sq[:, 0:128], True, False, [("ones_f",), ("sq",)], [("psn",)])
                self.mm(ps[PN][:, 256:384], self.ones_f[:, 0:128], sq[:, 128:256], False, True, [("ones_f",), ("sq",)], [("psn",)])
                self.actf(sdn, ps[PN][:, 256:384], AF.Ln, [("psn",), ("small",)], [("sdn",)], bias=self.eps, scale=1.0 / 256)
                self.actf(rs, sdn, AF.Exp, [("sdn",)], [("rs",)], scale=-0.5)
                for vc in range(2):
                    k = h2 * 2 + vc
                    self.stt(t1, ps[po][:, vc * 128:(vc + 1) * 128], self.gn[:, k + hp * 4:k + hp * 4 + 1], rs, ALU.mult, ALU.mult,
                             [("ps", po), ("gn",), ("rs",)], [("t1",)])
                    self.tt(ostage[:, k, tl], t1, grT[:, k, tl], ALU.mult, [("t1",), ("grT", k)], [("gost", k)])

        for i in range(NT):
            rec_A(i)
            if i >= 1:
                rec_N(i - 1)
        rec_N(NT - 1)
        r0 = 1024 + hp * 512
        self.dma("sp", "gost", self.mixT_d[r0:r0 + 512, :].rearrange("(k p) t -> p k t", p=128), ostage,
                 [("gost", k) for k in range(4)], [("mixT", 16 + hp)])
        A.release(m0)
        self.S.barrier()


    def phase5(self):
        A = self.A
        m0 = A.mark()
        mixS = self.xnT
        wo = v3(A.bf16(16 * D), D)
        xin = [A.f32(D), A.f32(D)]
        h1s = [A.f32(D), A.f32(D)]
        for q in range(4):
            self.dma("sp", f"mixS{q}", mixS[:, q * 4:(q + 1) * 4, :],
                     self.mixT_d[q * 512:(q + 1) * 512, :].rearrange("(kc p) t -> p kc t", p=128),
                     [("mixT", h) for h in range(18)], [("mixS", q)])
            self.dma("pool", f"wo{q}", wo[:, q * 4:(q + 1) * 4, :],
                     self.w_out_d[q * 512:(q + 1) * 512, :].rearrange("(kc p) n -> p kc n", p=128), [], [("wo", q)])
        for i in range(NT):
            s = i % 2
            self.dma("sp", f"xin{s}", xin[s], self.x_d[i * 128:(i + 1) * 128, :], [], [("xin", s)])
            for n in range(4):
                b = self.bank()
                for kc in range(16):
                    self.mm(self.ps[b][:, :], mixS[:, kc, i * 128:(i + 1) * 128], wo[:, kc, n * 512:(n + 1) * 512],
                            kc == 0, kc == 15, [("mixS", kc // 4), ("wo", kc // 4)], [("ps", b)])
                self.tt(h1s[s][:, n * 512:(n + 1) * 512], self.ps[b][:, :], xin[s][:, n * 512:(n + 1) * 512], ALU.add,
                        [("ps", b), ("xin", s)], [("h1s", s)])
            self.dma("sp", f"h1s{s}", self.h1_d[i * 128:(i + 1) * 128, :], h1s[s], [("h1s", s)], [("h1d", i)])
        A.release(m0)
        self.S.barrier()

    def bcap(self, ap, dims, off=0):
        return bass.AP(ap.tensor, ap.offset + off, [list(ap.ap[0])] + [list(d) for d in dims])

    def top16(self, vout, iout, src, tmp, rsrc, rv, ri, rtmp):
        self.S.dve(lambda e: e.max(out=vout[:, 0:8], in_=src), [rsrc], [rv])
        self.S.dve(lambda e: e.max_index(out=iout[:, 0:8], in_max=vout[:, 0:8], in_values=src), [rsrc, rv], [ri])
        self.S.dve(lambda e: e.match_replace(out=tmp, in_to_replace=vout[:, 0:8], in_values=src, imm_value=-1e30),
                   [rsrc, rv], [rtmp])
        self.S.dve(lambda e: e.max(out=vout[:, 8:16], in_=tmp), [rtmp], [rv])
        self.S.dve(lambda e: e.max_index(out=iout[:, 8:16], in_max=vout[:, 8:16], in_values=tmp), [rtmp, rv], [ri])

    def splice_plan(self, pops, nslots):
        plan = {}
        if not pops:
            return plan
        for cap in (0.2, 0.25, 0.3, 0.4, 0.5, 0.7, 1.0, 1.5, 3.0, 100.0):
            plan = {}
            lastw = {}
            budget = {}
            cur = 0
            for op in pops:
                eng = op["eng"]
                sl = cur
                for r in op["reads"] + op["writes"]:
                    w = lastw.get(r)
                    if w is None:
                        continue
                    if w[2]:
                        sl = max(sl, w[0] + 6)
                    elif w[1] != eng and w[0] >= sl:
                        sl = w[0] + 1
                c = op.get("cost", 0.12)
                if eng in ("dve", "act"):
                    while budget.get((sl, eng), 0.0) + c > cap and budget.get((sl, eng), 0.0) > 0:
                        sl += 1
                    budget[(sl, eng)] = budget.get((sl, eng), 0.0) + c
                cur = sl
                for r in op["writes"]:
                    lastw[r] = (sl, eng, op["dma"] is not None)
                plan.setdefault(sl, []).append(op)
            if cur < nslots:
                break
        if cur >= 128:
            merged = {}
            for sl, ops in sorted(plan.items()):
                merged.setdefault(min(sl, 127), []).extend(ops)
            plan = merged
        self.last_plan_len = cur
        return plan

    def capture(self, fn):
        saved = self.S.ops
        self.S.ops = []
        fn()
        got = self.S.ops
        self.S.ops = saved
        return got

    def table_cast(self, n):
        for _ in range(n):
            if not self._tc_pending:
                return
            src, c0, grp, nm, r0 = self._tc_pending.pop(0)
            self.dma("pool", grp, self.uv16_d[r0:r0 + 512, c0:c0 + D], src[r0:r0 + 512, :], [], [(nm,)])

    def peer(self):
        A = self.A
        NB = 7
        wq = v3(A.bf16(16 * D), D)
        k1T = A.f32(128)
        k2T = A.f32(128)
        kT = [k1T, k2T]
        identf = A.f32(128)
        gfc = A.f32(D)
        iota = A.f32(16)
        h1b = [A.f32(D) for _ in range(2)]
        hnb = [A.bf16(D) for _ in range(2)]
        junk = A.bf16(D)
        hnT = v3(A.bf16(16 * 128), 128)
        scrA = A.f32(2048)
        scrB = A.f32(2048)
        tmpm = A.f32(256)
        v16 = A.f32(256)
        i16 = A.u32(256)
        i16f = A.f32(256)
        tops = A.f32(128)
        pos = A.u32(128)
        pab = A.u32(128)
        paf = A.f32(128)
        pbf = A.f32(128)
        i1s = A.f32(128)
        i2s = A.f32(128)
        ef = A.f32(128)
        gs = A.f32(8)
        rg = A.f32(8)
        eidx = [A.i32(128) for _ in range(2)]
        gates = [A.f32(128) for _ in range(2)]
        actp = [A.f32(128) for _ in range(2)]
        gl = A.f32(128)
        wv = A.f32(128)
        dg = [A.bf16(128) for _ in range(4)]
        gb = [A.bf16(2 * D) for _ in range(NB)]
        ss2 = self.small[:, 64:80]
        sd2 = self.small[:, 80:96]
        rstd2 = self.small[:, 96:112]
        ss3 = self.small[:, 112:128]
        sd3 = self.small[:, 128:144]
        rstd3 = self.small[:, 144:160]
        ps = self.ps
        RB = [0, 1, 2, 3]
        ACC = [4, 5, 6, 7]
        rr = [0]

        def rbank():
            b = RB[rr[0] % 4]
            rr[0] += 1
            return b
        self.table_cast(1000)
        for q in range(4):
            self.dma("pool", f"wq{q}", wq[:, q * 4:(q + 1) * 4, :],
                     self.wq_d[q * 512:(q + 1) * 512, :].rearrange("(kc p) n -> p kc n", p=128), [], [("wq", q)])
        self.dma("sp", "gbc", self.gbc[:], self.ln2_d[0:1, :].partition_broadcast(128), [], [("gbc",)])
        self.dma("sp", "c_gfc", gfc, self.lnf_d[0:1, :].partition_broadcast(128), [], [("gfc",)])
        self.dma("sp", "c_iota", iota, self.c_iota16_d.ap(), [], [("iota",)])
        self.dma("sp", "c_k1", scrA[:, 0:128], self.k1_d.ap(), [], [("scrA",)])
        self.dma("sp", "c_k2", scrA[:, 128:256], self.k2_d.ap(), [], [("scrA",)])
        self.cp(identf, self.ident[:], [("ident",)], [("identf",)], eng="dve")
        for j in range(2):
            b = rbank()
            self.mm(ps[b][:, 0:128], scrA[:, j * 128:(j + 1) * 128], identf, True, True, [("scrA",), ("identf",)], [("ps", b)])
            self.cp(kT[j], ps[b][:, 0:128], [("ps", b)], [("kT",)], eng="dve")

        def C(c):
            self.S.default_cost = c

        def prep(i):
            s2 = i % 2
            s3 = s2
            tl = slice(i * 128, (i + 1) * 128)
            self.dma("sp", f"h1b{s3}", h1b[s3], self.h1_d[tl, :], [("h1d", i)], [("h1b", s3)])
            C(0.75)
            self.stt(junk, h1b[s3], 1.0, h1b[s3], ALU.mult, ALU.mult, [("h1b", s3)], [("junk",), ("ss2", i)],
                     accum_out=ss2[:, i:i + 1])
            C(0.12)
            self.actf(sd2[:, i:i + 1], ss2[:, i:i + 1], AF.Ln, [("ss2", i), ("small",)], [("sd2", i)], bias=self.eps, scale=1.0 / D)
            self.actf(rstd2[:, i:i + 1], sd2[:, i:i + 1], AF.Exp, [("sd2", i)], [("rstd2", i)], scale=-0.5)
            C(0.75)
            self.stt(hnb[s2], h1b[s3], rstd2[:, i:i + 1], self.gbc[:], ALU.mult, ALU.mult,
                     [("h1b", s3), ("rstd2", i), ("gbc",)], [("hnb", s2)])
            C(0.2)
            for g4 in range(4):
                b = rbank()
                for j in range(4):
                    kc = g4 * 4 + j
                    self.mm(ps[b][:, j * 128:(j + 1) * 128], hnb[s2][:, kc * 128:(kc + 1) * 128], self.ident[:], True, True,
                            [("hnb", s2), ("ident",)], [("ps", b)])
                self.cp(hnT[:, g4 * 4:(g4 + 1) * 4, :], v3(ps[b][:, :], 128), [("ps", b)], [("hnT",)],
                        eng=("act" if g4 % 2 == 0 else "dve"))
            qT = v3(scrA, 128)
            for g4 in range(4):
                b = rbank()
                for j in range(4):
                    m = g4 * 4 + j
                    for kc in range(16):
                        self.mm(ps[b][:, j * 128:(j + 1) * 128], wq[:, kc, m * 128:(m + 1) * 128], hnT[:, kc, :],
                                kc == 0, kc == 15, [("wq", kc // 4), ("hnT",)], [("ps", b)])
                self.cp(qT[:, g4 * 4:(g4 + 1) * 4, :], v3(ps[b][:, :], 128), [("ps", b)], [("scrA",)],
                        eng=("act" if g4 % 2 == 0 else "dve"))
            sc = v3(scrB, 128)
            for g4 in range(4):
                b = rbank()
                for j in range(4):
                    m = g4 * 4 + j
                    self.mm(ps[b][:, j * 128:(j + 1) * 128], qT[:, m, :], kT[m % 2], True, True, [("scrA",), ("kT",)], [("ps", b)])
                self.cp(sc[:, g4 * 4:(g4 + 1) * 4, :], v3(ps[b][:, :], 128), [("ps", b)], [("scrB",)],
                        eng=("act" if g4 % 2 == 0 else "dve"))
            C(0.12)
            v16v = v3(v16, 16)
            i16v = v3(i16, 16)
            for m in range(16):
                self.top16(v16v[:, m, :], i16v[:, m, :], sc[:, m, :], tmpm[:, 0:128], ("scrB",), ("v16",), ("i16",), ("tmpm",))
            cand = self.bcap(scrA, [[256, 8], [16, 16], [1, 16]])
            v1b = self.bcap(v16, [[32, 8], [1, 16], [0, 16]])
            v2b = self.bcap(v16, [[32, 8], [0, 16], [1, 16]], off=16)
            C(0.75)
            self.tt(cand, v1b, v2b, ALU.add, [("v16",)], [("scrA",)])
            C(0.15)
            cand3 = v3(scrA, 256)
            tops3 = v3(tops, 16)
            pos3 = v3(pos, 16)
            for h in range(8):
                self.top16(tops3[:, h, :], pos3[:, h, :], cand3[:, h, :], tmpm, ("scrA",), ("tops",), ("pos",), ("tmpm",))
            self.cp(i16f, i16, [("i16",)], [("i16f",)], eng="dve")
            self.S.dve(lambda e: e.tensor_single_scalar(pab, pos, 4, ALU.logical_shift_right), [("pos",)], [("pab",)])
            self.cp(paf, pab, [("pab",)], [("paf",)], eng="dve")
            self.S.dve(lambda e: e.tensor_single_scalar(pab, pos, 15, ALU.bitwise_and), [("pos",), ("paf",)], [("pab",)])
            self.cp(pbf, pab, [("pab",)], [("pbf",)], eng="dve")
            eq4 = self.bcap(scrB, [[256, 8], [16, 16], [1, 16]])
            C(0.75)
            iob = self.bcap(iota, [[0, 8], [0, 16], [1, 16]])
            for (pf, off, dst, nm) in ((paf, 0, i1s, "i1s"), (pbf, 16, i2s, "i2s")):
                pfb = self.bcap(pf, [[16, 8], [1, 16], [0, 16]])
                ifb = self.bcap(i16f, [[32, 8], [0, 16], [1, 16]], off=off)
                self.tt(eq4, pfb, iob, ALU.is_equal, [("paf",), ("pbf",), ("iota",)], [("scrB",)])
                self.tt(eq4, eq4, ifb, ALU.mult, [("scrB",), ("i16f",)], [("scrB",)])
                self.S.dve(lambda e, dst=dst: e.tensor_reduce(dst, v3(scrB, 16), AX.X, ALU.add), [("scrB",)], [(nm,)])
            C(0.12)
            self.stt(ef, i1s, 128.0, i2s, ALU.mult, ALU.add, [("i1s",), ("i2s",)], [("ef",)])
            self.cp(eidx[s3], ef, [("ef",)], [("eidx", s3)], eng="dve")
            g3 = v3(gates[s3], 16)
            self.tt(g3, tops3, self.bcap(tops, [[16, 8], [0, 16]]), ALU.subtract, [("tops",)], [("gates", s3)])
            self.actf(gates[s3], gates[s3], AF.Exp, [("gates", s3)], [("gates", s3)])
            self.S.dve(lambda e: e.tensor_reduce(gs, g3, AX.X, ALU.add), [("gates", s3)], [("gs",)])
            self.recip(rg, gs, [("gs",)], [("rg",)])
            self.tt(g3, g3, self.bcap(rg, [[1, 8], [0, 16]]), ALU.mult, [("gates", s3), ("rg",)], [("gates", s3)])
            if i == 0:
                self.tap("eidx0", eidx[0], [128, 128], I32, [("eidx", 0)])
                self.tap("gates0", gates[0], [128, 128], F32, [("gates", 0)])

        def step(i, j):
            s2 = i % 2
            k = (i * 128 + j) % NB
            if j == 0:
                self.memset(actp[s2], 0.0, [("actp", s2, jj) for jj in range(128)])
            self.S.dma("pool", f"gb{k}", lambda e: e.indirect_dma_start(
                out=gb[k], out_offset=None, in_=self.uv16_d[:, :],
                in_offset=bass.IndirectOffsetOnAxis(ap=eidx[s2][:, j:j + 1], axis=0)),
                [("eidx", s2), ("tabu",), ("tabv",)], [("gbu", k), ("gbv", k)])
            if j % 4 == 0 or (j < 24 and j % 2 == 0):
                self.stt(gb[k][:, 0:D], gb[k][:, 0:D], 1.0, hnb[s2], ALU.mult, ALU.mult, [("gbu", k), ("hnb", s2)],
                         [("gbu", k), ("actp", s2, j)], accum_out=actp[s2][:, j:j + 1])
            else:
                self.tt(gb[k][:, 0:D], gb[k][:, 0:D], hnb[s2], ALU.mult, [("gbu", k), ("hnb", s2)], [("gbu", k)])
                self.actf(gb[k][:, 0:D], gb[k][:, 0:D], AF.Identity, [("gbu", k)], [("gbu", k), ("actp", s2, j)],
                          accum_out=actp[s2][:, j:j + 1])
            self.actf(gl[:, j:j + 1], actp[s2][:, j:j + 1], AF.Gelu, [("actp", s2, j)], [("gl", j)])

        def step2(i, j):
            s2 = i % 2
            k = (i * 128 + j) % NB
            d = dg[j % 4]
            self.ts(d, self.ident[:], gl[:, j:j + 1], gates[s2][:, j:j + 1], ALU.mult, ALU.mult,
                    [("ident",), ("gl", j), ("gates", s2)], [("dg", j % 4)])
            for n in range(4):
                self.mm(ps[ACC[n]][:, :], d, gb[k][:, D + n * 512:D + (n + 1) * 512], j == 0, j == 127,
                        [("dg", j % 4), ("gbv", k)], [("ps", ACC[n])])

        def finish_acc(i):
            s2 = i % 2
            for n in range(4):
                self.tt(h1b[s2][:, n * 512:(n + 1) * 512], ps[ACC[n]][:, :], h1b[s2][:, n * 512:(n + 1) * 512], ALU.add,
                        [("ps", ACC[n]), ("h1b", s2)], [("h1b", s2)])

        def finish_tile(i):
            s2 = i % 2
            tl = slice(i * 128, (i + 1) * 128)
            C(0.75)
            self.stt(junk, h1b[s2], 1.0, h1b[s2], ALU.mult, ALU.mult, [("h1b", s2)], [("junk",), ("ss3", i)],
                     accum_out=ss3[:, i:i + 1])
            self.actf(sd3[:, i:i + 1], ss3[:, i:i + 1], AF.Ln, [("ss3", i), ("small",)], [("sd3", i)], bias=self.eps, scale=1.0 / D)
            self.actf(rstd3[:, i:i + 1], sd3[:, i:i + 1], AF.Exp, [("sd3", i)], [("rstd3", i)], scale=-0.5)
            self.stt(h1b[s2], h1b[s2], rstd3[:, i:i + 1], gfc, ALU.mult, ALU.mult, [("h1b", s2), ("rstd3", i), ("gfc",)], [("h1b", s2)])
            self.dma("sp", f"out{s2}", self.out_d[tl, :], h1b[s2], [("h1b", s2)], [("outd", i)])
            C(0.12)

        ntile = self.peer_tiles
        prep(0)
        for st in range(ntile):
            if st >= 1:
                finish_acc(st - 1)
            pops = self.capture(lambda: finish_tile(st - 1)) if st >= 1 else []
            pops += self.capture(lambda: prep(st + 1)) if st + 1 < ntile else []
            plan = self.splice_plan(pops, 120)
            for j in range(128):
                step(st, j)
                if j >= 1:
                    step2(st, j - 1)
                self.S.ops.extend(plan.get(j, []))
            step2(st, 127)
        finish_acc(ntile - 1)
        finish_tile(ntile - 1)

    def build(self):
        self.declare()
        W = self.w_in_d
        self.wplan = []
        for half in range(2):
            self.wplan += [(W, half * 512, 512), (W, 1024 + half * 512, 512), (W, 2048 + half * 512, 512)]
        self.wplan += [(W, 5120, 16)]
        for hp in range(2):
            self.wplan += [(W, 3072 + hp * 256, 256), (W, 3584 + hp * 256, 256), (W, 4096 + hp * 512, 512), (W, 5136 + hp * 512, 512)]
        self._wnext = 0
        self._wtaken = 0
        self.phase0()
        self.phase1()
        self.m_persist = self.A.mark()
        self.tap("xnT0", self.xnT[:, 0, :], [128, S], BF16, [("xnT", i) for i in range(NT)])
        if self.stop_after == "p1":
            return self.finish()
        for half in range(2):
            self.attention_half(half)
        if self.stop_after != "attn":
            self.gla_setup()
            for hp in range(2):
                self.gla_pair(hp)
        if self.stop_after in ("attn", "gla"):
            t = self.nc.dram_tensor("tap_mixT", [D, S], BF16, kind="ExternalOutput")
            self.tap_names.append("tap_mixT")
            nh = 16 if self.stop_after == "attn" else 18
            self.dma("sp", "tap_mixT", t.ap(), self.mixT_d.ap(), [("mixT", h) for h in range(nh)], [("tapd", "mixT")])
            return self.finish()
        self.A.release(self.m_persist)
        self.S.barrier()
        self.phase5()
        if self.stop_after == "h1":
            t = self.nc.dram_tensor("tap_h1", [S, D], F32, kind="ExternalOutput")
            self.tap_names.append("tap_h1")
            self.dma("sp", "tap_h1", t.ap(), self.h1_d.ap(), [("h1d", i) for i in range(NT)], [("tapd", "h1")])
            return self.finish()
        self.A.release(0)
        self.S.barrier()
        self.peer()
        return self.finish()

    def finish(self):
        nc = self.nc
        Sd = self.S
        Sd.analyse()
        es = self.es
        sems = {e: es.enter_context(nc.semaphore("s_" + e)) for e in ENGS}
        dsems = {g: es.enter_context(nc.semaphore("d_" + g)) for g in Sd.dma_count}
        with nc.Block() as block:
            @block.sync
            def _(e):
                known = Sd.emit_engine("sp", e, sems, dsems)
                Sd.final_waits("sp", e, sems, dsems, known)

            @block.scalar
            def _(e):
                Sd.emit_engine("act", e, sems, dsems)

            @block.vector
            def _(e):
                Sd.emit_engine("dve", e, sems, dsems)

            @block.gpsimd
            def _(e):
                Sd.emit_engine("pool", e, sems, dsems)

            @block.tensor
            def _(e):
                Sd.emit_engine("pe", e, sems, dsems)
        self.es.close()
        return nc


_CONSTS = None


def make_in_map(inputs, b):
    global _CONSTS
    if _CONSTS is None:
        _CONSTS = _consts()
    f = lambda a: np.ascontiguousarray(np.asarray(a, dtype=np.float32))
    m = {
        "x": f(inputs["x"][b]),
        "ln1_g": f(inputs["ln1_g"]).reshape(1, D),
        "w_in": f(inputs["w_in"][0]),
        "rel_bias": f(inputs["rel_bias"]),
        "gla_w_gate2": f(inputs["gla_w_gate2"][0]),
        "gla_b_gate": f(inputs["gla_b_gate"]).reshape(1, 512),
        "gla_norm_g": f(inputs["gla_norm_g"][0]),
        "w_out": f(inputs["w_out"][0]),
        "ln2_g": f(inputs["ln2_g"]).reshape(1, D),
        "peer_w_query": f(inputs["peer_w_query"][0]),
        "peer_keys1": f(inputs["peer_keys1"][0]),
        "peer_keys2": f(inputs["peer_keys2"][0]),
        "peer_u": f(inputs["peer_u"][0]),
        "peer_v": f(inputs["peer_v"][0]),
        "ln_f_g": f(inputs["ln_f_g"]).reshape(1, D),
    }
    m.update(_CONSTS)
    return m


_NC_CACHE = {}


def kernel(**inputs):
    if "nc" not in _NC_CACHE:
        _NC_CACHE["nc"] = Builder().build()
    nc = _NC_CACHE["nc"]
    n = 8
    in_maps = [make_in_map(inputs, b) for b in range(n)]
    res = run_bass_kernel_spmd(nc, in_maps, core_ids=list(range(n)))
    out = np.stack([np.asarray(r["out"], dtype=np.float32) for r in res.results], axis=0)
    return out
```
